# Optimizing a Trainium2 kernel written in Bass

```python
import math
import jax, jax.numpy as jnp
from jax import lax
import numpy as np

D_MODEL = 1024
BATCH = 8
SEQ = 2048
DEPTH = 2

CHUNK = 64
Q_BLOCK = 128
PLE_DIM = 256

A_HEADS = 6
A_DIM = 32
A_VDIM = 2 * A_DIM
B_HEADS = 6
B_Q_RANK = 256
B_KV_RANK = 128
B_NOPE = 64
B_ROPE = 32
B_VDIM = 64
ROPE_THETA = 10000.0
C_HEADS = 4
C_DIM = 64

MIX_A = A_HEADS * A_VDIM
MIX_B = B_HEADS * B_VDIM
MIX_C = C_HEADS * C_DIM
D_MIX = MIX_A + MIX_B + MIX_C
IN_SPLITS = (A_HEADS * 2 * A_DIM, A_HEADS * 2 * A_DIM, MIX_A,
             B_Q_RANK, B_KV_RANK, B_ROPE,
             MIX_C, MIX_C, MIX_C, C_HEADS)
D_IN = sum(IN_SPLITS)

N_GROUPS = 4
EXPERTS_PER_GROUP = 8
N_EXPERTS = N_GROUPS * EXPERTS_PER_GROUP
TOP_K_IN_GROUP = 2
D_EXPERT = 256

DEEPNORM_ALPHA = (2 * DEPTH) ** 0.25
DEEPNORM_BETA = (8 * DEPTH) ** -0.25
LN_EPS = 1e-5
RMS_EPS = 1e-6

kernel_name = "hymba_style_diff_mla_fox_hiermoe_deepnorm"


def _layer_norm(x, g, b):
    xf = x.astype(jnp.float32)
    mu = jnp.mean(xf, axis=-1, keepdims=True)
    var = jnp.mean(jnp.square(xf - mu), axis=-1, keepdims=True)
    y = (xf - mu) * lax.rsqrt(var + LN_EPS) * g.astype(jnp.float32) + b.astype(jnp.float32)
    return y.astype(x.dtype)


def _rms_norm(x, g):
    xf = x.astype(jnp.float32)
    y = xf * lax.rsqrt(jnp.mean(jnp.square(xf), axis=-1, keepdims=True) + RMS_EPS)
    return (y * g.astype(jnp.float32)).astype(x.dtype)


def _rope_tables(pos):
    half = B_ROPE // 2
    inv = ROPE_THETA ** (-jnp.arange(half, dtype=jnp.float32) / half)
    ang = pos.astype(jnp.float32)[..., None] * inv
    return jnp.cos(ang), jnp.sin(ang)


def _rope(x, cos, sin):
    half = x.shape[-1] // 2
    x1, x2 = x[..., :half], x[..., half:]
    c, s = cos.astype(x.dtype), sin.astype(x.dtype)
    return jnp.concatenate([x1 * c - x2 * s, x2 * c + x1 * s], axis=-1)


def _alibi_slopes(n):
    return 2.0 ** (-8.0 * jnp.arange(1, n + 1, dtype=jnp.float32) / n)


def _to_heads(t, n_heads):
    b, s, _ = t.shape
    return t.reshape(b, s, n_heads, -1).transpose(0, 2, 1, 3)


def _from_heads(t):
    b, h, s, d = t.shape
    return t.transpose(0, 2, 1, 3).reshape(b, s, h * d)


def _block_attention(q, k, v, scale, bias_fn, per_frame_causal):
    seq = q.shape[2]
    outs = []
    for t0 in range(0, seq, Q_BLOCK):
        t1 = t0 + Q_BLOCK
        logits = jnp.einsum('bhqd,bhkd->bhqk', q[:, :, t0:t1], k[:, :, :t1],
                            preferred_element_type=jnp.float32) * scale
        if bias_fn is not None:
            logits = logits + bias_fn(t0, t1)
        t_idx = jnp.arange(t0, t1)[:, None]
        s_idx = jnp.arange(t1)[None, :]
        allowed = (s_idx <= t_idx) if per_frame_causal else (s_idx // CHUNK <= t_idx // CHUNK)
        logits = jnp.where(allowed, logits, -jnp.inf)
        probs = jax.nn.softmax(logits, axis=-1).astype(v.dtype)
        outs.append(jnp.einsum('bhqk,bhkd->bhqd', probs, v[:, :, :t1]))
    return jnp.concatenate(outs, axis=2)


def _token_mixers(x, layer, pos, cos, sin, w_in, w_uq, w_ukv, g_cq, g_ckv,
                  lam_q1, lam_k1, lam_q2, lam_k2, g_diff, b_forget, w_out):
    b, s, _ = x.shape
    z = x @ w_in
    qa, ka, va, cq, ckv, kr, qc, kc, vc, fc = jnp.split(
        z, np.cumsum(IN_SPLITS)[:-1].tolist(), axis=-1)

    qa = qa.reshape(b, s, A_HEADS, 2, A_DIM)
    ka = ka.reshape(b, s, A_HEADS, 2, A_DIM)
    qa1, qa2 = qa[:, :, :, 0].transpose(0, 2, 1, 3), qa[:, :, :, 1].transpose(0, 2, 1, 3)
    ka1, ka2 = ka[:, :, :, 0].transpose(0, 2, 1, 3), ka[:, :, :, 1].transpose(0, 2, 1, 3)
    v_a = _to_heads(va, A_HEADS)
    slopes = _alibi_slopes(A_HEADS)
    posf = pos.astype(jnp.float32)

    def alibi(t0, t1):
        dist = jnp.abs(posf[:, None, t0:t1, None] - posf[:, None, None, :t1])
        return -slopes[None, :, None, None] * dist

    scale_a = 1.0 / math.sqrt(A_DIM)
    o_a1 = _block_attention(qa1, ka1, v_a, scale_a, alibi, False)
    o_a2 = _block_attention(qa2, ka2, v_a, scale_a, alibi, False)
    lam_init = 0.8 - 0.6 * math.exp(-0.3 * layer)
    lam = (jnp.exp(jnp.sum(lam_q1.astype(jnp.float32) * lam_k1.astype(jnp.float32)))
           - jnp.exp(jnp.sum(lam_q2.astype(jnp.float32) * lam_k2.astype(jnp.float32))) + lam_init)
    o_a = o_a1 - lam.astype(o_a1.dtype) * o_a2
    o_a = _rms_norm(o_a, g_diff) * (1.0 - lam_init)

    c_q = _rms_norm(cq, g_cq)
    q_b = (c_q @ w_uq).reshape(b, s, B_HEADS, B_NOPE + B_ROPE)
    q_rope = _rope(q_b[..., B_NOPE:], cos[:, :, None], sin[:, :, None])
    q_b = jnp.concatenate([q_b[..., :B_NOPE], q_rope], axis=-1)
    c_kv = _rms_norm(ckv, g_ckv)
    kv_b = (c_kv @ w_ukv).reshape(b, s, B_HEADS, B_NOPE + B_VDIM)
    k_nope, v_b = kv_b[..., :B_NOPE], kv_b[..., B_NOPE:]
    k_rope = _rope(kr, cos, sin)
    k_b = jnp.concatenate(
        [k_nope, jnp.broadcast_to(k_rope[:, :, None], (b, s, B_HEADS, B_ROPE))], axis=-1)
    o_b = _block_attention(q_b.transpose(0, 2, 1, 3), k_b.transpose(0, 2, 1, 3),
                           v_b.transpose(0, 2, 1, 3), 1.0 / math.sqrt(B_NOPE + B_ROPE),
                           None, False)

    log_f = jax.nn.log_sigmoid(fc.astype(jnp.float32) + b_forget.astype(jnp.float32))
    cum = jnp.cumsum(log_f, axis=1).transpose(0, 2, 1)

    def decay(t0, t1):
        return cum[:, :, t0:t1, None] - cum[:, :, None, :t1]

    o_c = _block_attention(_to_heads(qc, C_HEADS), _to_heads(kc, C_HEADS),
                           _to_heads(vc, C_HEADS), 1.0 / math.sqrt(C_DIM), decay, True)

    mixed = jnp.concatenate([_from_heads(o_a), _from_heads(o_b), _from_heads(o_c)], axis=-1)
    return mixed @ w_out


def _hier_moe(h, w_group, b_group, w_erouter, b_erouter, w_gate_e, w_up_e, w_down_e):
    b, s, d = h.shape
    xt = h.reshape(b * s, d)
    g_logits = (xt @ w_group).astype(jnp.float32) + b_group.astype(jnp.float32)
    g_prob = jax.nn.softmax(g_logits, axis=-1)
    _, g_idx = lax.top_k(g_logits, 1)
    g_w = jnp.take_along_axis(g_prob, g_idx, axis=-1)
    e_all = jnp.einsum('nd,gde->nge', xt, w_erouter).astype(jnp.float32) + b_erouter.astype(jnp.float32)
    e_logits = jnp.einsum('ng,nge->ne', jax.nn.one_hot(g_idx[:, 0], N_GROUPS, dtype=jnp.float32), e_all)
    top_val, top_idx = lax.top_k(e_logits, TOP_K_IN_GROUP)
    e_w = jax.nn.softmax(top_val, axis=-1) * g_w
    expert_id = g_idx * EXPERTS_PER_GROUP + top_idx
    combine = jnp.sum(jax.nn.one_hot(expert_id, N_EXPERTS, dtype=jnp.float32) * e_w[..., None], axis=1)
    gate = jnp.einsum('nd,edf->nef', xt, w_gate_e)
    up = jnp.einsum('nd,edf->nef', xt, w_up_e)
    act = jax.nn.silu(gate) * up * combine[:, :, None].astype(xt.dtype)
    y = jnp.einsum('nef,efd->nd', act, w_down_e)
    return y.reshape(b, s, d)


def setup_inputs(seed: int = 0) -> dict:
    key = jax.random.key(seed)
    ks = iter(jax.random.split(key, 32))

    def nrm(shape, scale):
        return jax.random.normal(next(ks), shape, jnp.float32) * scale

    def gain(shape):
        return 1.0 + nrm(shape, 0.02)

    x = nrm((BATCH, SEQ, D_MODEL), 1.0)
    p = nrm((DEPTH, BATCH, SEQ, PLE_DIM), 1.0)
    offset = jax.random.randint(next(ks), (BATCH,), 0, 64, jnp.int32) * CHUNK
    positions = offset[:, None] + jnp.arange(SEQ, dtype=jnp.int32)[None, :]
    return {
        "x": x,
        "p": p,
        "positions": positions,
        "w_in": nrm((DEPTH, D_MODEL, D_IN), D_MODEL ** -0.5),
        "w_uq": nrm((DEPTH, B_Q_RANK, B_HEADS * (B_NOPE + B_ROPE)), B_Q_RANK ** -0.5),
        "w_ukv": nrm((DEPTH, B_KV_RANK, B_HEADS * (B_NOPE + B_VDIM)), B_KV_RANK ** -0.5),
        "g_cq": gain((DEPTH, B_Q_RANK)),
        "g_ckv": gain((DEPTH, B_KV_RANK)),
        "lam_q1": nrm((DEPTH, A_DIM), 0.1),
        "lam_k1": nrm((DEPTH, A_DIM), 0.1),
        "lam_q2": nrm((DEPTH, A_DIM), 0.1),
        "lam_k2": nrm((DEPTH, A_DIM), 0.1),
        "g_diff": gain((DEPTH, A_VDIM)),
        "b_forget": 2.0 + nrm((DEPTH, C_HEADS), 0.1),
        "w_out": nrm((DEPTH, D_MIX, D_MODEL), D_MIX ** -0.5 * DEEPNORM_BETA),
        "ln1_g": gain((DEPTH, D_MODEL)),
        "ln1_b": nrm((DEPTH, D_MODEL), 0.02),
        "w_group": nrm((DEPTH, D_MODEL, N_GROUPS), D_MODEL ** -0.5),
        "b_group": nrm((DEPTH, N_GROUPS), 0.01),
        "w_erouter": nrm((DEPTH, N_GROUPS, D_MODEL, EXPERTS_PER_GROUP), D_MODEL ** -0.5),
        "b_erouter": nrm((DEPTH, N_GROUPS, EXPERTS_PER_GROUP), 0.01),
        "w_gate_e": nrm((DEPTH, N_EXPERTS, D_MODEL, D_EXPERT), D_MODEL ** -0.5),
        "w_up_e": nrm((DEPTH, N_EXPERTS, D_MODEL, D_EXPERT), D_MODEL ** -0.5),
        "w_down_e": nrm((DEPTH, N_EXPERTS, D_EXPERT, D_MODEL), D_EXPERT ** -0.5 * DEEPNORM_BETA),
        "w_ple_gate": nrm((DEPTH, D_MODEL, D_MODEL), D_MODEL ** -0.5),
        "w_ple_proj": nrm((DEPTH, PLE_DIM, D_MODEL), PLE_DIM ** -0.5 * DEEPNORM_BETA),
        "ln2_g": gain((DEPTH, D_MODEL)),
        "ln2_b": nrm((DEPTH, D_MODEL), 0.02),
    }


def reference(x, p, positions, w_in, w_uq, w_ukv, g_cq, g_ckv, lam_q1, lam_k1, lam_q2, lam_k2,
              g_diff, b_forget, w_out, ln1_g, ln1_b, w_group, b_group, w_erouter, b_erouter,
              w_gate_e, w_up_e, w_down_e, w_ple_gate, w_ple_proj, ln2_g, ln2_b):
    cos, sin = _rope_tables(positions)
    for i in range(DEPTH):
        mix = _token_mixers(x, i, positions, cos, sin, w_in[i], w_uq[i], w_ukv[i], g_cq[i],
                            g_ckv[i], lam_q1[i], lam_k1[i], lam_q2[i], lam_k2[i], g_diff[i],
                            b_forget[i], w_out[i])
        h = _layer_norm(DEEPNORM_ALPHA * x + mix, ln1_g[i], ln1_b[i])
        moe = _hier_moe(h, w_group[i], b_group[i], w_erouter[i], b_erouter[i],
                        w_gate_e[i], w_up_e[i], w_down_e[i])
        ple = jax.nn.sigmoid(h @ w_ple_gate[i]) * (p[i].astype(h.dtype) @ w_ple_proj[i])
        x = _layer_norm(DEEPNORM_ALPHA * h + moe + ple, ln2_g[i], ln2_b[i])
    return x
```

```python
import numpy as np
import concourse.bass as bass
import concourse.mybir as mybir
from concourse.bass_utils import run_bass_kernel_spmd
from contextlib import ExitStack

F32, BF16, I32 = mybir.dt.float32, mybir.dt.bfloat16, mybir.dt.int32
AF = mybir.ActivationFunctionType
ALU = mybir.AluOpType
AX = mybir.AxisListType


class Tracker:
    ENGS = ['pe', 'act', 'dve', 'pool', 'sp']

    def __init__(self, nc, es, serialize=False, same_engine_sync=True, ndma=16):
        self.nc = nc
        self.h = {'pe': nc.tensor, 'act': nc.scalar, 'dve': nc.vector, 'pool': nc.gpsimd, 'sp': nc.sync}
        self.sem = {e: es.enter_context(nc.semaphore("s_" + e)) for e in self.ENGS}
        self.ndma = ndma
        self.dsem = {q: [es.enter_context(nc.semaphore("d%s%d" % (q, i))) for i in range(ndma)] for q in ('sp', 'pool')}
        self.ops = []
        self.lastw = {}
        self.readers = {}
        self.serialize = serialize
        self.same_engine_sync = same_engine_sync
        self.out_dmas = []
        self.do_schedule = True

    def op(self, eng, fn, reads=(), writes=(), dma=False, is_out=False, cost=0.3, lat=0.0):
        idx = len(self.ops)
        deps = set()
        psr = [k for k in reads if isinstance(k, tuple) and k[0] == 'ps']
        if psr:
            reads = [k for k in reads if not (isinstance(k, tuple) and k[0] == 'ps')]
            writes = list(writes) + psr
        for k in reads:
            w = self.lastw.get(k)
            if w is not None:
                deps.add(w)
        for k in writes:
            w = self.lastw.get(k)
            if w is not None:
                deps.add(w)
            rd = self.readers.get(k)
            if rd:
                deps.update(rd)
        if self.serialize and idx > 0:
            deps.add(idx - 1)
        for k in writes:
            self.lastw[k] = idx
            self.readers[k] = []
        for k in reads:
            self.readers.setdefault(k, []).append(idx)
        deps.discard(idx)
        self.ops.append([eng, fn, deps, dma, False, None, None, cost, lat])
        if is_out:
            self.out_dmas.append(idx)
        return idx

    def dma(self, eng, out, in_, reads=(), writes=(), is_out=False, **kw):
        nbytes = 4.0
        for d in in_.shape:
            nbytes *= d
        return self.op(eng, lambda h: h.dma_start(out=out, in_=in_, **kw), reads, writes, dma=True, is_out=is_out,
                       cost=(1.0 if eng == 'pool' else 0.15), lat=2.0 + nbytes / 120e3)

    def schedule(self, window=8000):
        import heapq
        ops = self.ops
        n = len(ops)
        ndep = [len(o[2]) for o in ops]
        users = [[] for _ in range(n)]
        for i, o in enumerate(ops):
            for d in o[2]:
                users[d].append(i)
        ready_t = [0.0] * n
        fin = [0.0] * n
        heaps = {e: [] for e in self.ENGS}
        for i in range(n):
            if ndep[i] == 0:
                heapq.heappush(heaps[ops[i][0]], (i, i))
        free = {e: 0.0 for e in self.ENGS}
        order = []
        done = [False] * n
        low = 0
        while len(order) < n:
            while low < n and done[low]:
                low += 1
            best = None
            for e in self.ENGS:
                h = heaps[e]
                cands = []
                while h and len(cands) < 160:
                    c = heapq.heappop(h)
                    cands.append(c)
                pick = None
                for c in cands:
                    i = c[1]
                    if i > low + window:
                        continue
                    st = max(free[e], ready_t[i])
                    if pick is None or st < pick[0] - 1e-9:
                        pick = (st, i)
                for c in cands:
                    heapq.heappush(h, c)
                if pick is not None and (best is None or pick[0] < best[0] - 1e-9 or (abs(pick[0] - best[0]) <= 1e-9 and pick[1] < best[1])):
                    best = (pick[0], pick[1], e)
            if best is None:
                best_i = None
                for e in self.ENGS:
                    if heaps[e]:
                        i = heaps[e][0][1]
                        if best_i is None or i < best_i:
                            best_i = i
                e = ops[best_i][0]
                best = (max(free[e], ready_t[best_i]), best_i, e)
            st, i, e = best
            h = heaps[e]
            h.remove((i, i))
            heapq.heapify(h)
            o = ops[i]
            free[e] = st + o[7]
            fin[i] = st + o[7] + o[8]
            done[i] = True
            order.append(i)
            for u in users[i]:
                ndep[u] -= 1
                lat = 0.25 if ops[u][0] != e else 0.15
                if fin[i] + lat > ready_t[u]:
                    ready_t[u] = fin[i] + lat
                if ndep[u] == 0:
                    heapq.heappush(heaps[ops[u][0]], (u, u))
        self.est_time = max(fin) if fin else 0.0
        return order

    def barrier(self):
        self.op('sp', None, reads=(), writes=('__bar__',))
        self.ops[-1][1] = 'BARRIER'

    def emit(self):
        ops = self.ops
        ENGS = self.ENGS
        since = []
        bar = None
        for i, o in enumerate(ops):
            if o[1] == 'BARRIER':
                o[2] = set(since)
                if bar is not None:
                    o[2].add(bar)
                since = []
                bar = i
                continue
            if bar is not None:
                o[2].add(bar)
            since.append(i)
        for o in ops:
            for d in o[2]:
                p = ops[d]
                if (not p[3]) and p[0] == o[0] and (o[0] == 'pe' or not self.same_engine_sync) and not o[3]:
                    continue
                p[4] = True
        order = self.schedule() if self.do_schedule else list(range(len(ops)))
        cnt = {e: 0 for e in ENGS}
        seen = {e: {} for e in ENGS}
        dval = {q: [0] * self.ndma for q in ('sp', 'pool')}
        nissued = {'sp': 0, 'pool': 0}
        ndma_issued = 0
        nwait = 0
        for oi in order:
            o = ops[oi]
            eng, fn, deps, isdma, signal = o[0], o[1], o[2], o[3], o[4]
            h = self.h[eng]
            waits = {}
            for d in deps:
                p = ops[d]
                if p[3]:
                    sem, val = p[5], p[6]
                else:
                    if p[0] == eng and (eng == 'pe' or not self.same_engine_sync) and not isdma:
                        continue
                    sem, val = self.sem[p[0]], p[6]
                    if val is None:
                        raise RuntimeError("dep on non-signaling op")
                key = id(sem)
                if key not in waits or waits[key][1] < val:
                    waits[key] = (sem, val)
            for key, (sem, val) in waits.items():
                if seen[eng].get(key, 0) >= val:
                    continue
                h.wait_ge(sem, val)
                nwait += 1
                seen[eng][key] = val
            if fn == 'BARRIER':
                cnt[eng] += 1
                h.sem_inc(self.sem[eng], 1)
                o[6] = cnt[eng]
                continue
            if isdma:
                i = nissued[eng] % self.ndma
                nissued[eng] += 1
                ndma_issued += 1
                sem = self.dsem[eng][i]
                dv = dval[eng]
                if dv[i] > 0 and seen[eng].get(id(sem), 0) < dv[i]:
                    h.wait_ge(sem, dv[i])
                    seen[eng][id(sem)] = dv[i]
                ins = fn(h)
                ins.then_inc(sem, 16)
                dv[i] += 16
                o[5], o[6] = sem, dv[i]
            else:
                ins = fn(h)
                if signal:
                    cnt[eng] += 1
                    ins.then_inc(self.sem[eng], 1)
                    o[6] = cnt[eng]
        h = self.h['sp']
        for d in self.out_dmas:
            p = ops[d]
            h.wait_ge(p[5], p[6])
        self.stats = dict(nops=len(ops), nwait=nwait, cnt=cnt, ndma=ndma_issued)


S = 2048
DM = 1024
NT = 16
DEPTH = 2
ALPHA = float((2 * DEPTH) ** 0.25)
C_QA, C_KA, C_VA, C_CQ, C_CKV, C_KR, C_QC, C_KC, C_VC, C_FC = 0, 384, 768, 1152, 1408, 1536, 1568, 1824, 2080, 2336
SLOPES = [float(2.0 ** (-8.0 * (i + 1) / 6)) for i in range(6)]

WNAMES = [("w_in", [DEPTH, 1024, 2340]), ("w_uq", [DEPTH, 256, 576]), ("w_ukv", [DEPTH, 128, 768]),
          ("g_cq", [DEPTH, 256]), ("g_ckv", [DEPTH, 128]), ("lam_q1", [DEPTH, 32]), ("lam_k1", [DEPTH, 32]),
          ("lam_q2", [DEPTH, 32]), ("lam_k2", [DEPTH, 32]), ("g_diff", [DEPTH, 64]), ("b_forget", [DEPTH, 4]),
          ("w_out", [DEPTH, 1024, 1024]), ("ln1_g", [DEPTH, 1024]), ("ln1_b", [DEPTH, 1024]),
          ("w_group", [DEPTH, 1024, 4]), ("b_group", [DEPTH, 4]), ("w_erouter", [DEPTH, 4, 1024, 8]),
          ("b_erouter", [DEPTH, 32]), ("w_gate_e", [DEPTH, 32, 1024, 256]), ("w_up_e", [DEPTH, 32, 1024, 256]),
          ("w_down_e", [DEPTH, 32, 256, 1024]), ("w_ple_gate", [DEPTH, 1024, 1024]),
          ("w_ple_proj", [DEPTH, 256, 1024]), ("ln2_g", [DEPTH, 1024]), ("ln2_b", [DEPTH, 1024])]


def build(nlayers=DEPTH, serialize=False, same_engine_sync=True, dbg_names=(), stop_after=None):
    nc = bass.Bass("TRN2", target_bir_lowering=False)
    x_d = nc.dram_tensor("x", [S, DM], F32, kind="ExternalInput").ap()
    p_d = nc.dram_tensor("p", [DEPTH, S, 256], F32, kind="ExternalInput").ap()
    pos_d = nc.dram_tensor("positions", [S], I32, kind="ExternalInput").ap()
    cst_d = nc.dram_tensor("cst", [16], F32, kind="ExternalInput").ap()
    W = {n: nc.dram_tensor(n, sh, F32, kind="ExternalInput").ap() for n, sh in WNAMES}
    out_d = nc.dram_tensor("out", [S, DM], F32, kind="ExternalOutput").ap()
    dbg_d = {}

    top = ExitStack()
    with top:
        T = Tracker(nc, top, serialize=serialize, same_engine_sync=same_engine_sync)

        uid = [0]

        def sbt(es, name, shape, dt):
            uid[0] += 1
            return es.enter_context(nc.sbuf_tensor("%s_%d" % (name, uid[0]), shape, dt))

        def dbg(name, ap, reads, shape):
            if name not in dbg_names:
                return
            d = nc.dram_tensor("dbg_" + name, list(shape), ap.dtype, kind="ExternalOutput").ap()
            dbg_d[name] = d
            T.dma('sp', d, ap, reads=reads, is_out=True)

        R = sbt(top, "R", [128, NT, DM], F32)
        XT = sbt(top, "XT", [128, 8, S], BF16)
        MT = sbt(top, "MT", [128, 8, S], BF16)
        ident_f = sbt(top, "ident_f", [128, 128], F32)
        ident_b = sbt(top, "ident_b", [128, 128], BF16)
        posk = sbt(top, "posk", [128, NT], F32)
        negposk = sbt(top, "negposk", [128, NT], F32)
        cc_t = sbt(top, "cc_t", [128, NT, 32], F32)
        ss_t = sbt(top, "ss_t", [128, NT, 32], F32)
        posmask = sbt(top, "posmask", [128, 128], F32)
        one1 = sbt(top, "one1", [128, 1], F32)
        slp6 = sbt(top, "slp6", [128, 6], F32)
        PF = [top.enter_context(nc.psum_tensor("pf%d" % i, [128, 512], F32)) for i in range(8)]
        PK = [('ps', i) for i in range(8)]

        def pbv(i):
            return PF[i][:].bitcast(BF16)

        RK = lambda t: ('R', t)
        XK = lambda t: ('XT', t)
        MK = lambda c, t: ('MT', c, t)

        def fsize(ap):
            n = 1
            for d in ap.shape[1:]:
                n *= d
            return n

        def V(eng, method, reads, writes, *a, **kw):
            o = kw.get('out', a[0] if a else None)
            n = fsize(o) if o is not None else 64
            if eng == 'act':
                c = 0.3 + n / 1400.0
            elif eng == 'dve':
                c = 0.22 + n / 960.0
            else:
                c = 0.35 + n / 420.0
            T.op(eng, lambda h: getattr(h, method)(*a, **kw), reads, writes, cost=c)

        def mm(out, lhsT, rhs, start, stop, reads, writes, **kw):
            n = max(fsize(out), 64)
            kk = lhsT.shape[0]
            f32 = 4.0 if lhsT.dtype == F32 else 1.0
            c = 0.012 + (n / 2400.0) * (1.0 + 1.4 * (128 - kk) / 96.0 if kk < 128 else 1.0) * f32 + (0.03 if n < 256 else 0.0)
            T.op('pe', lambda h: h.matmul(out, lhsT, rhs, start=start, stop=stop, **kw), reads, writes, cost=c)

        def tr(out, in_, reads, writes):
            ident = ident_b if in_.dtype == BF16 else ident_f
            T.op('pe', lambda h: h.transpose(out=out, in_=in_, identity=ident[:]), list(reads) + ['ident'], writes, cost=0.09)

        with ExitStack() as es0:
            posk_i = sbt(es0, "posk_i", [128, NT], I32)
            cst = sbt(es0, "cst_sb", [128, 16], F32)
            ang = sbt(es0, "ang", [128, NT, 16], F32)
            angi = sbt(es0, "angi", [128, NT, 16], I32)
            angf = sbt(es0, "angf", [128, NT, 16], F32)
            angg = sbt(es0, "angg", [128, NT, 16], F32)
            T.dma('sp', posk_i[:], pos_d.rearrange("(t p) -> p t", p=128), writes=['posk_i'],
                  allow_slow_non_contiguous=True)
            T.dma('sp', cst[:], cst_d.partition_broadcast(128), writes=['cst'])
            V('dve', 'tensor_copy', ['posk_i'], ['posk'], out=posk[:], in_=posk_i[:])
            V('dve', 'tensor_scalar', ['posk'], ['negposk'], out=negposk[:], in0=posk[:], scalar1=-1.0, scalar2=None, op0=ALU.mult)
            V('pool', 'memset', [], ['ident'], ident_f[:], 1.0)
            V('pool', 'memset', [], ['one1'], one1[:], 1.0)
            for i in range(6):
                V('pool', 'memset', ['slp6'] if i else [], ['slp6'], slp6[:, i:i + 1], SLOPES[i] * float(np.sqrt(32.0)))
            V('pool', 'affine_select', ['ident'], ['ident'], out=ident_f[:], in_=ident_f[:], pattern=[[-1, 128]],
              compare_op=ALU.is_equal, fill=0.0, base=0, channel_multiplier=1)
            V('dve', 'tensor_copy', ['ident'], ['ident'], out=ident_b[:], in_=ident_f[:])
            V('pool', 'memset', [], ['posmask'], posmask[:], 0.0)
            V('pool', 'affine_select', ['posmask'], ['posmask'], out=posmask[:], in_=posmask[:], pattern=[[1, 128]],
              compare_op=ALU.is_ge, fill=30000.0, base=0, channel_multiplier=-1)

            def table(dst_lo, dst_hi, phase, sign_lo, sign_hi):
                V('dve', 'tensor_tensor', ['posk', 'cst'], ['ang'], out=ang[:],
                  in0=posk[:].unsqueeze(2).to_broadcast([128, NT, 16]),
                  in1=cst[:, 0:16].unsqueeze(1).to_broadcast([128, NT, 16]), op=ALU.mult)
                if phase != 0.0:
                    V('dve', 'tensor_scalar', ['ang'], ['ang'], out=ang[:], in0=ang[:], scalar1=phase, scalar2=None,
                      op0=ALU.add)
                V('dve', 'tensor_copy', ['ang'], ['angi'], out=angi[:], in_=ang[:])
                V('dve', 'tensor_copy', ['angi'], ['angf'], out=angf[:], in_=angi[:])
                V('dve', 'tensor_tensor', ['ang', 'angf'], ['ang'], out=ang[:], in0=ang[:], in1=angf[:], op=ALU.subtract)
                V('dve', 'tensor_scalar', ['ang'], ['angg'], out=angg[:], in0=ang[:], scalar1=0.5, scalar2=None,
                  op0=ALU.is_gt)
                V('dve', 'tensor_tensor', ['ang', 'angg'], ['ang'], out=ang[:], in0=ang[:], in1=angg[:], op=ALU.subtract)
                V('dve', 'tensor_scalar', ['ang'], ['angg'], out=angg[:], in0=ang[:], scalar1=-0.5, scalar2=None,
                  op0=ALU.is_lt)
                V('dve', 'tensor_tensor', ['ang', 'angg'], ['ang'], out=ang[:], in0=ang[:], in1=angg[:], op=ALU.add)
                V('act', 'activation', ['ang'], ['angf'], out=angf[:], in_=ang[:], func=AF.Sin, scale=float(2 * np.pi))
                V('dve', 'tensor_scalar', ['angf'], ['tab'], out=dst_lo, in0=angf[:], scalar1=sign_lo, scalar2=None,
                  op0=ALU.mult)
                V('dve', 'tensor_scalar', ['angf'], ['tab'], out=dst_hi, in0=angf[:], scalar1=sign_hi, scalar2=None,
                  op0=ALU.mult)
            table(cc_t[:, :, 0:16], cc_t[:, :, 16:32], 0.25, 1.0, 1.0)
            table(ss_t[:, :, 0:16], ss_t[:, :, 16:32], 0.0, -1.0, 1.0)
            T.barrier()
        dbg("cc", cc_t[:, :, :], ['tab'], [128, NT, 32])
        dbg("ss", ss_t[:, :, :], ['tab'], [128, NT, 32])

        for t in range(NT):
            T.dma('sp', R[:, t, :], x_d[t * 128:(t + 1) * 128, :], writes=[RK(t)])

        def build_xt(es, tag):
            xb = [sbt(es, "xb%s%d" % (tag, i), [128, DM], BF16) for i in range(2)]
            for t in range(NT):
                b = xb[t % 2]
                bk = ('xb', t % 2)
                V('act', 'copy', [RK(t)], [bk], out=b[:], in_=R[:, t, :])
                pi = 6 + (t % 2)
                for c in range(8):
                    tr(pbv(pi)[:, c * 128:(c + 1) * 128], b[:, c * 128:(c + 1) * 128], [bk], [PK[pi]])
                V('dve', 'tensor_copy', [PK[pi]], [XK(t)], out=XT[:, :, t * 128:(t + 1) * 128],
                  in_=pbv(pi).rearrange("p (c k) -> p c k", c=8))

        def wload(dst, src, key):
            T.dma('pool', dst, src, writes=[key])

        def win_cols(l, c0, n):
            return W["w_in"][l].rearrange("(c p) n -> p c n", p=128)[:, :, c0:c0 + n]

        bank_rr = [0]

        def nextbank(choices):
            bank_rr[0] += 1
            return choices[bank_rr[0] % len(choices)]

        def proj_fm(Wt, wkey, col0, M, evac, banks=(0, 1, 6, 7)):
            for tg in range(4):
                bi = nextbank(banks)
                for c in range(8):
                    mm(PF[bi][0:M, :], Wt[:, c, col0:col0 + M], XT[:, c, tg * 512:(tg + 1) * 512], c == 0, c == 7,
                       [wkey] + [XK(4 * tg + i) for i in range(4)], [PK[bi]])
                evac(tg, bi)

        def proj_tm(Wt, wkey, col0, N, evac, banks=(0, 1, 6, 7)):
            for t in range(NT):
                bi = nextbank(banks)
                for c in range(8):
                    mm(PF[bi][:, 0:N], XT[:, c, t * 128:(t + 1) * 128], Wt[:, c, col0:col0 + N], c == 0, c == 7,
                       [wkey, XK(t)], [PK[bi]])
                evac(t, bi)

        def attend(sc, heads, mode, scale, obanks, finalize, stbanks=(0, 1, 7), look=2):
            assert look + 1 <= len(sc['PT']) and look + 1 <= len(stbanks) + 0
            from collections import deque
            PT, TB, DT = sc['PT'], sc['TB'], sc['DT']
            posq = sc.get('posq')
            cnt = sc['cnt']
            pending = deque()

            def retire():
                x = pending.popleft()
                x()

            for qg in range(4):
                for kt in range(4 * qg + 4):
                    q0 = max(qg * 512, kt * 128)
                    n = (qg + 1) * 512 - q0
                    diag = kt >= 4 * qg
                    dk = None
                    di = 0
                    if mode == 'A':
                        di = cnt[2] % 3
                        cnt[2] += 1
                        dk = ('DT', di)
                        V('pool', 'tensor_tensor', ['posq', 'posk'], [dk], out=DT[di][:, 0:n], in0=posq[:, q0:q0 + n],
                          in1=posk[:, kt:kt + 1].to_broadcast([128, n]), op=ALU.subtract)
                        V('dve', 'scalar_tensor_tensor', [dk], [dk], out=DT[di][:, 0:n], in0=DT[di][:, 0:n], scalar=-1.0,
                          in1=DT[di][:, 0:n], op0=ALU.mult, op1=ALU.min)
                    for hi, hd in enumerate(heads):
                        si = stbanks[cnt[0] % len(stbanks)]
                        cnt[0] += 1
                        pi = cnt[1] % 6
                        ti = cnt[1] % len(TB)
                        cnt[1] += 1
                        ptk = ('PT', pi)
                        tbk = ('TB', ti)
                        mm(PF[si][:, 0:n], hd['KT'](kt), hd['QT'](q0, n), True, mode != 'A', hd['rk'](kt, qg), [PK[si]],
                           **hd.get('kw', {}))
                        if mode == 'A':
                            bk = ('BH', di, hi // 2)
                            if hi % 2 == 0:
                                V('dve', 'tensor_scalar', [dk], [bk], out=sc['BH'][di][hi // 2][:, 0:n], in0=DT[di][:, 0:n],
                                  scalar1=hd['slope'], scalar2=None, op0=ALU.mult)
                            mm(PF[si][:, 0:n], ident_b[:, :], sc['BH'][di][hi // 2][:, 0:n], False, True, [bk, 'ident'], [PK[si]])
                            V('act', 'activation', [PK[si]], [ptk], out=PT[pi][:, 0:n], in_=PF[si][:, 0:n], func=AF.Exp,
                              scale=scale)
                        elif mode == 'B':
                            V('act', 'activation', [PK[si]], [ptk], out=PT[pi][:, 0:n], in_=PF[si][:, 0:n], func=AF.Exp,
                              scale=scale)
                        else:
                            V('dve', 'scalar_tensor_tensor', [hd['cqk'], 'cumk', PK[si]], [tbk], out=TB[ti][:, 0:n],
                              in0=hd['cumq'][:, q0:q0 + n], scalar=hd['cumk'](kt), in1=PF[si][:, 0:n],
                              op0=ALU.subtract, op1=ALU.subtract)
                            if diag:
                                V('dve', 'tensor_tensor', [tbk, 'posmask'], [tbk], out=TB[ti][:, 0:128],
                                  in0=TB[ti][:, 0:128], in1=posmask[:], op=ALU.add)
                            V('act', 'activation', [tbk], [ptk], out=PT[pi][:, 0:n], in_=TB[ti][:, 0:n], func=AF.Exp,
                              scale=-1.0)
                        if diag and mode != 'C':
                            V('pool', 'memset', [ptk], [ptk], PT[pi][64:128, 0:64], 0.0)
                        ob = obanks[hi]

                        def pv(kt=kt, q0=q0, n=n, qg=qg, pi=pi, ptk=ptk, hd=hd, ob=ob):
                            for j in range(n // 128):
                                qt = q0 // 128 + j
                                jj = qt - 4 * qg
                                mm(PF[ob][:, jj * 128:jj * 128 + 65], PT[pi][:, j * 128:(j + 1) * 128], hd['V'](kt),
                                   kt == 0 and j == 0, kt == qt, [ptk] + hd['vk'](kt), [PK[ob]], skip_group_check=True)
                        pending.append(pv)
                        while len(pending) > look:
                            retire()
                pending.append(lambda qg=qg: finalize(qg))
            while pending:
                retire()

        def norm_out(ob, dst, dkeys, rden):
            Ov = PF[ob][:, :].rearrange("p (j e) -> p j e", e=128)
            V('dve', 'reciprocal', [PK[ob]], ['rden'], out=rden[:], in_=Ov[:, :, 64])
            V('dve', 'tensor_tensor', [PK[ob], 'rden'], dkeys, out=dst, in0=Ov[:, :, 0:64],
              in1=rden[:].unsqueeze(2).to_broadcast([128, 4, 64]), op=ALU.mult)

        def flush_chunk(mixed, mkey, chunk, qg, pi=6):
            for j in range(4):
                tr(pbv(pi)[:, j * 128:(j + 1) * 128], mixed[:, j, :], [mkey], [PK[pi]])
            V('act', 'copy', [PK[pi]], [MK(chunk, 4 * qg + i) for i in range(4)],
              out=MT[:, chunk, qg * 512:(qg + 1) * 512], in_=pbv(pi)[:, 0:512])

        def layer_norm(es, gname, bname, l, tag, after=None, ntmp=2):
            g_b = sbt(es, "lng" + tag, [128, DM], F32)
            b_b = sbt(es, "lnb" + tag, [128, DM], F32)
            st = sbt(es, "lnst" + tag, [128, 2, 6], F32)
            ag = sbt(es, "lnag" + tag, [128, 2], F32)
            rs = sbt(es, "lnrs" + tag, [128, 1], F32)
            nb_ = sbt(es, "lnnb" + tag, [128, 1], F32)
            tmp = [sbt(es, "lntmp%s%d" % (tag, i), [128, DM], F32) for i in range(ntmp)]
            T.dma('sp', g_b[:], W[gname][l].partition_broadcast(128), writes=['lng'])
            T.dma('sp', b_b[:], W[bname][l].partition_broadcast(128), writes=['lnb'])
            for t in range(NT):
                tk = ('lntmp', t % ntmp)
                tm = tmp[t % ntmp]
                V('dve', 'bn_stats', [RK(t)], ['lnst'], out=st[:, 0, :], in_=R[:, t, 0:512])
                V('dve', 'bn_stats', [RK(t), 'lnst'], ['lnst'], out=st[:, 1, :], in_=R[:, t, 512:1024])
                V('dve', 'bn_aggr', ['lnst'], ['lnag'], out=ag[:], in_=st[:])
                V('act', 'activation', ['lnag'], ['lnrs'], out=rs[:], in_=ag[:, 1:2], func=AF.Sqrt, bias=1e-5, scale=1.0)
                V('dve', 'reciprocal', ['lnrs'], ['lnrs'], out=rs[:], in_=rs[:])
                V('dve', 'scalar_tensor_tensor', ['lnag', 'lnrs'], ['lnnb'], out=nb_[:], in0=ag[:, 0:1], scalar=-1.0, in1=rs[:],
                  op0=ALU.mult, op1=ALU.mult)
                V('act', 'activation', [RK(t), 'lnnb', 'lnrs'], [tk], out=tm[:], in_=R[:, t, :], func=AF.Identity,
                  scale=rs[:, 0:1], bias=nb_[:, 0:1])
                V('dve', 'tensor_tensor', [tk, 'lng'], [tk], out=tm[:], in0=tm[:], in1=g_b[:], op=ALU.mult)
                V('pool', 'tensor_tensor', [tk, 'lnb'], [RK(t)], out=R[:, t, :], in0=tm[:], in1=b_b[:], op=ALU.add)
                if after is not None:
                    after(t)

        done = False
        for l in range(nlayers):
            if done:
                break
            lam_init = 0.8 - 0.6 * float(np.exp(-0.3 * l))
            with ExitStack() as esA:
                if l == 0:
                    with ExitStack() as esx:
                        build_xt(esx, "a%d" % l)
                        T.barrier()
                dbg("xt%d" % l, XT[:, :, :], [XK(t) for t in range(NT)], [128, 8, S])
                sc = dict(PT=[sbt(esA, "PT%d" % i, [128, 512], BF16) for i in range(6)], TB=None, DT=None, cnt=[0, 0, 0])
                rden = sbt(esA, "rden", [128, 4], F32)
                mixed = [sbt(esA, "mixed%d" % i, [128, 4, 128], BF16) for i in range(2)]
                lamt = sbt(esA, "lamt", [128, 4, 32], F32)
                lamp = sbt(esA, "lamp", [128, 2, 32], F32)
                lams = sbt(esA, "lams", [128, 2], F32)
                neglam = sbt(esA, "neglam", [128, 1], F32)
                gdiff = sbt(esA, "gdiff", [128, 64], F32)
                for i, nm in enumerate(["lam_q1", "lam_k1", "lam_q2", "lam_k2"]):
                    T.dma('sp', lamt[:, i, :], W[nm][l].partition_broadcast(128), writes=['lamt%d' % i])
                T.dma('sp', gdiff[:], W["g_diff"][l].partition_broadcast(128), writes=['gdiff'])
                V('dve', 'tensor_tensor', ['lamt0', 'lamt1'], ['lamp'], out=lamp[:, 0, :], in0=lamt[:, 0, :], in1=lamt[:, 1, :],
                  op=ALU.mult)
                V('dve', 'tensor_tensor', ['lamt2', 'lamt3', 'lamp'], ['lamp'], out=lamp[:, 1, :], in0=lamt[:, 2, :],
                  in1=lamt[:, 3, :], op=ALU.mult)
                V('dve', 'reduce_sum', ['lamp'], ['lams'], out=lams[:], in_=lamp[:], axis=AX.X)
                V('act', 'activation', ['lams'], ['lams'], out=lams[:], in_=lams[:], func=AF.Exp)
                V('dve', 'tensor_tensor', ['lams'], ['neglam'], out=neglam[:], in0=lams[:, 1:2], in1=lams[:, 0:1],
                  op=ALU.subtract)
                V('dve', 'tensor_scalar', ['neglam'], ['neglam'], out=neglam[:], in0=neglam[:], scalar1=-lam_init,
                  scalar2=None, op0=ALU.add)
                V('dve', 'tensor_scalar', ['gdiff'], ['gdiff'], out=gdiff[:], in0=gdiff[:], scalar1=1.0 - lam_init,
                  scalar2=None, op0=ALU.mult)

                with ExitStack() as es:
                    sc['TB'] = [None]
                    posq = sbt(es, "posq", [128, S], F32)
                    with ExitStack() as esp:
                        posq_i = sbt(esp, "posq_i", [128, S], I32)
                        T.dma('sp', posq_i[:], pos_d.partition_broadcast(128), writes=['posq_i'])
                        V('dve', 'tensor_copy', ['posq_i'], ['posq'], out=posq[:], in_=posq_i[:])
                        T.barrier()
                    sc['posq'] = posq
                    sc['DT'] = [sbt(es, "DT%d" % i, [128, 512], F32) for i in range(3)]
                    sc['BH'] = [[sbt(es, "BH%d_%d" % (i, j), [128, 512], BF16) for j in range(2)] for i in range(3)]
                    WA = sbt(es, "WA", [128, 8, 384], BF16)
                    QA4 = [sbt(es, "QA%d" % i, [128, S], BF16) for i in range(4)]
                    for i in range(4):
                        if i % 2 == 0:
                            V('act', 'memzero', [], [('QA', i, tg) for tg in range(4)], QA4[i][:])
                        else:
                            V('dve', 'memset', [], [('QA', i, tg) for tg in range(4)], QA4[i][:], 0.0)
                    KA = sbt(es, "KA", [128, S], BF16)
                    VA = sbt(es, "VA", [128, NT, 2, 65], BF16)
                    oA = [sbt(es, "oA%d" % i, [128, 4, 64], F32) for i in range(4)]
                    dA = sbt(es, "dA", [128, 4, 64], F32)
                    sqA = sbt(es, "sqA", [128, 4, 64], F32)
                    ssA = sbt(es, "ssA", [128, 4], F32)
                    V('pool', 'memset', [], [('VA', t) for t in range(NT)], VA[:, :, :, 64:65], 1.0)
                    scale_a = 1.0 / float(np.sqrt(32.0))
                    for b in range(3):
                        for i, c0 in enumerate([C_QA, C_KA, C_VA]):
                            wload(WA[:, :, i * 128:(i + 1) * 128], win_cols(l, c0 + b * 128, 128), 'WA%d' % i)
                        def evac_qa(tg, bi):
                            for i in range(4):
                                V('act' if i % 2 == 0 else 'dve', 'copy' if i % 2 == 0 else 'tensor_copy', [PK[bi]], [('QA', i, tg)],
                                  out=QA4[i][32 * i:32 * i + 32, tg * 512:(tg + 1) * 512], in_=PF[bi][32 * i:32 * i + 32, :])
                        proj_fm(WA, 'WA0', 0, 128, evac_qa)
                        proj_fm(WA, 'WA1', 128, 128, lambda tg, bi: V('act', 'copy', [PK[bi]], [('KA', tg)],
                                out=KA[:, tg * 512:(tg + 1) * 512], in_=PF[bi][:, :]))
                        proj_tm(WA, 'WA2', 256, 128, lambda t, bi: V('dve', 'tensor_copy', [PK[bi]], [('VA', t)],
                                out=VA[:, t, :, 0:64], in_=PF[bi][:, 0:128].rearrange("p (h e) -> p h e", h=2)))
                        if b == 0:
                            dbg("va%d" % l, VA[:, :, :, :], [('VA', i) for i in range(NT)], [128, NT, 2, 65])
                        heads = []
                        for hl in range(2):
                            for m in range(2):
                                p0 = hl * 64 + m * 32
                                heads.append(dict(
                                    KT=lambda kt: KA[:, kt * 128:(kt + 1) * 128],
                                    QT=lambda q0, n, i=p0 // 32: QA4[i][:, q0:q0 + n],
                                    V=lambda kt, hl=hl: VA[:, kt, hl, :],
                                    rk=lambda kt, qg, i=p0 // 32: [('KA', kt // 4), ('QA', i, qg)],
                                    vk=lambda kt: [('VA', kt)],
                                    slope=SLOPES[2 * b + hl] / scale_a, h=2 * b + hl))
                        mx = mixed[b % 2]
                        mkey = ('mixed', b % 2)

                        def fin_a(qg, b=b, mx=mx, mkey=mkey):
                            for hi in range(4):
                                norm_out(2 + hi, oA[hi][:], [('oA', hi)], rden)
                            for hl in range(2):
                                o1, o2 = oA[2 * hl], oA[2 * hl + 1]
                                V('dve', 'scalar_tensor_tensor', [('oA', 2 * hl), ('oA', 2 * hl + 1), 'neglam'], ['dA'],
                                  out=dA[:], in0=o2[:], scalar=neglam[:, 0:1], in1=o1[:], op0=ALU.mult, op1=ALU.add)
                                V('pool', 'tensor_tensor', ['dA'], ['sqA'], out=sqA[:], in0=dA[:], in1=dA[:], op=ALU.mult)
                                V('dve', 'reduce_sum', ['sqA'], ['ssA'], out=ssA[:], in_=sqA[:], axis=AX.X)
                                V('act', 'activation', ['ssA'], ['ssA'], out=ssA[:], in_=ssA[:], func=AF.Sqrt,
                                  scale=1.0 / 64.0, bias=1e-6)
                                V('dve', 'reciprocal', ['ssA'], ['ssA'], out=ssA[:], in_=ssA[:])
                                V('dve', 'tensor_tensor', ['dA', 'ssA'], ['sqA'], out=sqA[:], in0=dA[:],
                                  in1=ssA[:].unsqueeze(2).to_broadcast([128, 4, 64]), op=ALU.mult)
                                V('pool', 'tensor_tensor', ['sqA', 'gdiff'], [mkey], out=mx[:, :, hl * 64:(hl + 1) * 64],
                                  in0=sqA[:], in1=gdiff[:].unsqueeze(1).to_broadcast([128, 4, 64]), op=ALU.mult)
                            flush_chunk(mx, mkey, b, qg)
                        attend(sc, heads, 'A', scale_a, [2, 3, 4, 5], fin_a, stbanks=(0, 1, 6, 7), look=3)
                    T.barrier()
                dbg("mtA%d" % l, MT[:, 0:3, :], [MK(c, t) for c in range(3) for t in range(NT)], [128, 3, S])
                if stop_after == ('A', l):
                    done = True

                if not done:
                  with ExitStack() as es:
                    WB = sbt(es, "WB", [128, 8, 416], BF16)
                    Wuq = sbt(es, "Wuq", [128, 2, 576], BF16)
                    Wukv = sbt(es, "Wukv", [128, 768], BF16)
                    gq_b = sbt(es, "gq_b", [128, 256], F32)
                    gkv_b = sbt(es, "gkv_b", [128, 128], F32)
                    cqT = sbt(es, "cqT", [128, 2, S], BF16)
                    ckvT = sbt(es, "ckvT", [128, S], BF16)
                    QBs = [sbt(es, "QB%d" % i, [128, 2, S], BF16) for i in range(2)]
                    KBs = [sbt(es, "KB%d" % i, [128, 2, S], BF16) for i in range(2)]
                    VBs = [sbt(es, "VB%d" % i, [128, NT, 2, 65], BF16) for i in range(1)] * 2
                    for i in range(2):
                        V('act', 'memzero', [], [('QB', i, j) for j in range(4)], QBs[i][96:128, :, :])
                        V('dve', 'memset', [], [('KB', i, j) for j in range(4)], KBs[i][96:128, :, :], 0.0)
                        if i == 0:
                            V('pool', 'memset', [], [('VB', 0, t) for t in range(NT)], VBs[0][:, :, :, 64:65], 1.0)
                    stats4 = [sbt(es, "statB%d" % i, [128, 4], F32) for i in range(4)]
                    junks = [sbt(es, "junkB%d" % i, [128, 256], F32) for i in range(1)] * 2
                    cqn = [sbt(es, "cqn%d" % i, [128, 384], BF16) for i in range(2)] * 2
                    krs = [sbt(es, "krs%d" % i, [128, 96], BF16) for i in range(4)]
                    rt1s = [sbt(es, "rt1%d" % i, [128, 2, 32], F32) for i in range(2)]
                    rt2s = [sbt(es, "rt2%d" % i, [128, 2, 32], F32) for i in range(2)]
                    qst = [sbt(es, "qst%d" % i, [128, 2, 96], BF16) for i in range(2)]
                    wload(WB[:], win_cols(l, C_CQ, 416), 'WB')
                    wload(Wuq[:], W["w_uq"][l].rearrange("(c p) n -> p c n", p=128), 'Wuq')
                    wload(Wukv[:], W["w_ukv"][l], 'Wukv')
                    T.dma('sp', gq_b[:], W["g_cq"][l].partition_broadcast(128), writes=['gq_b'])
                    T.dma('sp', gkv_b[:], W["g_ckv"][l].partition_broadcast(128), writes=['gkv_b'])
                    for i in range(4):
                        V('pool', 'memset', [], [('krs', i)], krs[i][:], 0.0)

                    def evac_b1(t, bi):
                        pb = PF[bi]
                        r4 = t % 4
                        stat = stats4[r4]
                        sk_ = lambda i: ('statB', r4, i)
                        jk = ('junkB', 0)
                        jn = junks[t % 2]
                        V('pool', 'memset', [], [sk_(0), sk_(1)], stat[:, 0:2], 0.0)
                        V('act', 'activation', [PK[bi]], [jk, sk_(0)], out=jn[:, 0:256], in_=pb[:, 0:256],
                          func=AF.Square, accum_out=stat[:, 0:1])
                        V('act', 'activation', [PK[bi], jk], [jk, sk_(1)], out=jn[:, 0:128], in_=pb[:, 256:384],
                          func=AF.Square, accum_out=stat[:, 1:2])
                        V('act', 'activation', [sk_(0)], [sk_(2)], out=stat[:, 2:3], in_=stat[:, 0:1], func=AF.Sqrt,
                          scale=1.0 / 256.0, bias=1e-6)
                        V('act', 'activation', [sk_(1)], [sk_(3)], out=stat[:, 3:4], in_=stat[:, 1:2], func=AF.Sqrt,
                          scale=1.0 / 128.0, bias=1e-6)
                        V('dve', 'reciprocal', [sk_(2), sk_(3)], [sk_(4)], out=stat[:, 2:4], in_=stat[:, 2:4])
                        cn = cqn[r4]
                        ck = ('cqn', r4 % 2)
                        V('dve', 'scalar_tensor_tensor', [PK[bi], sk_(4), 'gq_b'], [ck], out=cn[:, 0:256], in0=pb[:, 0:256],
                          scalar=stat[:, 2:3], in1=gq_b[:], op0=ALU.mult, op1=ALU.mult)
                        V('dve', 'scalar_tensor_tensor', [PK[bi], sk_(4), 'gkv_b', ck], [ck], out=cn[:, 256:384],
                          in0=pb[:, 256:384], scalar=stat[:, 3:4], in1=gkv_b[:], op0=ALU.mult, op1=ALU.mult)
                        ks = krs[r4]
                        kk = ('krs', r4)
                        r1, r2 = rt1s[t % 2], rt2s[t % 2]
                        r1k, r2k = ('rt1', t % 2), ('rt2', t % 2)
                        V('dve', 'tensor_tensor', [PK[bi], 'tab'], [r1k], out=r1[:, 0, :], in0=pb[:, 384:416],
                          in1=cc_t[:, t, :], op=ALU.mult)
                        V('dve', 'tensor_tensor', [PK[bi], 'tab'], [r2k], out=r2[:, 0, 0:16], in0=pb[:, 400:416],
                          in1=ss_t[:, t, 0:16], op=ALU.mult)
                        V('dve', 'tensor_tensor', [PK[bi], 'tab', r2k], [r2k], out=r2[:, 0, 16:32], in0=pb[:, 384:400],
                          in1=ss_t[:, t, 16:32], op=ALU.mult)
                        V('pool', 'tensor_tensor', [r1k, r2k, kk], [kk], out=ks[:, 64:96], in0=r1[:, 0, :],
                          in1=r2[:, 0, :], op=ALU.add)
                        pi = 4 + (t % 4)
                        for c in range(3):
                            tr(pbv(pi)[:, c * 128:(c + 1) * 128], cn[:, c * 128:(c + 1) * 128], [ck], [PK[pi]])
                        tr(pbv(pi)[0:96, 384:512], ks[:, :], [kk], [PK[pi]])
                        V('dve', 'tensor_copy', [PK[pi]], [('cqT', t)], out=cqT[:, :, t * 128:(t + 1) * 128],
                          in_=pbv(pi)[:, 0:256].rearrange("p (c k) -> p c k", c=2))
                        V('dve', 'tensor_copy', [PK[pi]], [('ckvT', t)], out=ckvT[:, t * 128:(t + 1) * 128], in_=pbv(pi)[:, 256:384])
                        for i in range(2):
                            V('dve', 'tensor_copy', [PK[pi]], [('KBr', i, t)], out=KBs[i][64:96, :, t * 128:(t + 1) * 128],
                              in_=pbv(pi)[64:96, 384:512].unsqueeze(1).to_broadcast([32, 2, 128]))
                    proj_tm(WB, 'WB', 0, 416, evac_b1, banks=(0, 1, 2, 3))
                    dbg("cqT%d" % l, cqT[:, :, :], [('cqT', t) for t in range(NT)], [128, 2, S])
                    scale_b = 1.0 / float(np.sqrt(96.0))
                    for b in range(3):
                        if stop_after == ('B1', l):
                            done = True
                            break
                        h0 = 2 * b
                        QB, KB, VB = QBs[b % 2], KBs[b % 2], VBs[b % 2]
                        bb = b % 2
                        for t in range(NT):
                            bi = nextbank((0, 1, 6, 7))
                            for c in range(2):
                                mm(PF[bi][:, 0:192], cqT[:, c, t * 128:(t + 1) * 128], Wuq[:, c, h0 * 96:h0 * 96 + 192],
                                   c == 0, c == 1, ['Wuq', ('cqT', t)], [PK[bi]])
                            qv = PF[bi][:, 0:192].rearrange("p (h e) -> p h e", h=2)
                            qs = qst[t % 2]
                            qk = ('qst', t % 2)
                            rt1, rt2 = rt1s[t % 2], rt2s[t % 2]
                            r1k, r2k = ('rt1', t % 2), ('rt2', t % 2)
                            V('dve', 'tensor_copy', [PK[bi]], [qk], out=qs[:, :, 0:64], in_=qv[:, :, 0:64])
                            V('dve', 'tensor_tensor', [PK[bi], 'tab'], [r1k], out=rt1[:], in0=qv[:, :, 64:96],
                              in1=cc_t[:, t, :].unsqueeze(1).to_broadcast([128, 2, 32]), op=ALU.mult)
                            V('dve', 'tensor_tensor', [PK[bi], 'tab'], [r2k], out=rt2[:, :, 0:16], in0=qv[:, :, 80:96],
                              in1=ss_t[:, t, 0:16].unsqueeze(1).to_broadcast([128, 2, 16]), op=ALU.mult)
                            V('dve', 'tensor_tensor', [PK[bi], 'tab', r2k], [r2k], out=rt2[:, :, 16:32], in0=qv[:, :, 64:80],
                              in1=ss_t[:, t, 16:32].unsqueeze(1).to_broadcast([128, 2, 16]), op=ALU.mult)
                            V('pool', 'tensor_tensor', [r1k, r2k, qk], [qk], out=qs[:, :, 64:96], in0=rt1[:], in1=rt2[:],
                              op=ALU.add)
                            pi = 6 + (t % 2)
                            for hl in range(2):
                                tr(pbv(pi)[0:96, hl * 128:(hl + 1) * 128], qs[:, hl, :], [qk], [PK[pi]])
                            V('dve', 'tensor_copy', [PK[pi]], [('QB', bb, t // 4)], out=QB[0:96, :, t * 128:(t + 1) * 128],
                              in_=pbv(pi)[0:96, 0:256].rearrange("p (h k) -> p h k", h=2))
                        for hl in range(2):
                            h = h0 + hl
                            for tg in range(4):
                                bi = nextbank((0, 1, 6, 7))
                                mm(PF[bi][0:64, :], Wukv[:, h * 128:h * 128 + 64], ckvT[:, tg * 512:(tg + 1) * 512], True, True,
                                   ['Wukv'] + [('ckvT', 4 * tg + i) for i in range(4)], [PK[bi]])
                                V('dve', 'tensor_copy', [PK[bi]], [('KB', bb, tg)], out=KB[0:64, hl, tg * 512:(tg + 1) * 512],
                                  in_=PF[bi][0:64, :])
                        for t in range(NT):
                            bi = nextbank((0, 1, 6, 7))
                            for hl in range(2):
                                h = h0 + hl
                                mm(PF[bi][:, hl * 64:(hl + 1) * 64], ckvT[:, t * 128:(t + 1) * 128],
                                   Wukv[:, h * 128 + 64:h * 128 + 128], True, True, ['Wukv', ('ckvT', t)], [PK[bi]])
                            V('dve', 'tensor_copy', [PK[bi]], [('VB', 0, t)], out=VB[:, t, :, 0:64],
                              in_=PF[bi][:, 0:128].rearrange("p (h e) -> p h e", h=2))
                        if stop_after == ('B2', l):
                            done = True
                            break
                        heads = []
                        for hl in range(2):
                            heads.append(dict(
                                KT=lambda kt, hl=hl, KB=KB: KB[:, hl, kt * 128:(kt + 1) * 128],
                                QT=lambda q0, n, hl=hl, QB=QB: QB[:, hl, q0:q0 + n],
                                V=lambda kt, hl=hl, VB=VB: VB[:, kt, hl, :],
                                rk=lambda kt, qg, bb=bb: [('KB', bb, kt // 4), ('QB', bb, qg)] + [('KBr', bb, 4 * (kt // 4) + i) for i in range(4)],
                                vk=lambda kt: [('VB', 0, kt)]))
                        mx = mixed[b % 2]
                        mkey = ('mixed', b % 2)

                        def fin_b(qg, b=b, mx=mx, mkey=mkey):
                            for hl in range(2):
                                norm_out(2 + hl, mx[:, :, hl * 64:(hl + 1) * 64], [mkey], rden)
                            flush_chunk(mx, mkey, 3 + b, qg)
                        attend(sc, heads, 'B', scale_b, [2, 3], fin_b, stbanks=(0, 1, 4, 5, 6, 7), look=5)
                    T.barrier()
                  dbg("mtB%d" % l, MT[:, 3:6, :], [MK(c, t) for c in range(3, 6) for t in range(NT)], [128, 3, S])
                if stop_after == ('B', l):
                    done = True

                if not done:
                  with ExitStack() as es:
                    sc['TB'] = [sbt(es, "TBc%d" % i, [128, 512], F32) for i in range(4)]
                    WC = sbt(es, "WC", [128, 8, 388], BF16)
                    QC2 = [sbt(es, "QC%d" % i, [128, S], BF16) for i in range(2)]
                    V('act', 'memzero', [], [('QC', 0, tg) for tg in range(4)], QC2[0][:])
                    V('dve', 'memset', [], [('QC', 1, tg) for tg in range(4)], QC2[1][:], 0.0)
                    KC = sbt(es, "KC", [128, S], BF16)
                    VC = sbt(es, "VC", [128, NT, 2, 65], BF16)
                    cumneg = sbt(es, "cumneg", [4, S], F32)
                    negb = sbt(es, "negb", [4, 1], F32)
                    sel4 = sbt(es, "sel4", [4, 4, 128], F32)
                    cumk = sbt(es, "cumk", [128, NT, 4], F32)
                    cumq = None
                    V('pool', 'memset', [], [('VC', t) for t in range(NT)], VC[:, :, :, 64:65], 1.0)
                    V('dve', 'tensor_copy', ['ident'], ['sel4'], out=sel4[:],
                      in_=ident_f[0:4, 0:4].unsqueeze(2).to_broadcast([4, 4, 128]))
                    T.dma('sp', negb[:], W["b_forget"][l].rearrange("(h o) -> h o", o=1), writes=['negb'])
                    V('dve', 'tensor_scalar', ['negb'], ['negb'], out=negb[:], in0=negb[:], scalar1=-1.0, scalar2=None,
                      op0=ALU.mult)
                    for b in range(2):
                        for i, c0 in enumerate([C_QC, C_KC, C_VC]):
                            wload(WC[:, :, i * 128:(i + 1) * 128], win_cols(l, c0 + b * 128, 128), 'WC%d' % i)
                        if b == 0:
                            wload(WC[:, :, 384:388], win_cols(l, C_FC, 4), 'WC3')
                            esf = ExitStack()
                            lf = sbt(esf, "lf", [4, S], F32)
                            def evac_f(tg, bi):
                                V('act', 'activation', [PK[bi], 'negb'], [('lf', tg)], out=lf[:, tg * 512:(tg + 1) * 512],
                                  in_=PF[bi][0:4, :], func=AF.Exp, scale=-1.0, bias=negb[:, 0:1])
                                V('act', 'activation', [('lf', tg)], [('lf', tg)], out=lf[:, tg * 512:(tg + 1) * 512],
                                  in_=lf[:, tg * 512:(tg + 1) * 512], func=AF.Ln, bias=1.0)
                            proj_fm(WC, 'WC3', 384, 4, evac_f)
                            V('dve', 'tensor_tensor_scan', [('lf', i) for i in range(4)] + ['one1'], ['cumneg'],
                              out=cumneg[:], data0=one1[0:4, 0:1].to_broadcast([4, S]), data1=lf[:], initial=0.0, op0=ALU.mult, op1=ALU.add)
                            dbg("cumneg%d" % l, cumneg[:, :], ['cumneg'], [4, S])
                            bi = nextbank((0, 1, 6, 7))
                            for t in range(NT):
                                mm(PF[bi][:, t * 4:(t + 1) * 4], cumneg[0:4, t * 128:(t + 1) * 128], ident_f[0:4, 0:4],
                                   True, True, ['cumneg', 'ident'], [PK[bi]])
                            V('dve', 'tensor_copy', [PK[bi]], ['cumk'], out=cumk[:],
                              in_=PF[bi][:, 0:64].rearrange("p (t h) -> p t h", h=4))
                            esf.close()
                            cumq = [sbt(es, "cumq%d" % i, [128, S], F32) for i in range(2)]
                        for hl in range(2):
                            for tg in range(4):
                                bi = nextbank((0, 1, 6, 7))
                                mm(PF[bi][:, :], sel4[0:4, 2 * b + hl, :], cumneg[0:4, tg * 512:(tg + 1) * 512], True, True,
                                   ['sel4', 'cumneg'], [PK[bi]])
                                V('dve', 'tensor_copy', [PK[bi]], [('cumq', hl)], out=cumq[hl][:, tg * 512:(tg + 1) * 512],
                                  in_=PF[bi][:, :])
                        def evac_qc(tg, bi):
                            for i in range(2):
                                V('act', 'mul', [PK[bi]], [('QC', i, tg)], out=QC2[i][64 * i:64 * i + 64, tg * 512:(tg + 1) * 512],
                                  in_=PF[bi][64 * i:64 * i + 64, :], mul=0.125)
                        proj_fm(WC, 'WC0', 0, 128, evac_qc)
                        proj_fm(WC, 'WC1', 128, 128, lambda tg, bi: V('act', 'copy', [PK[bi]], [('KC', tg)],
                                out=KC[:, tg * 512:(tg + 1) * 512], in_=PF[bi][:, :]))
                        proj_tm(WC, 'WC2', 256, 128, lambda t, bi: V('dve', 'tensor_copy', [PK[bi]], [('VC', t)],
                                out=VC[:, t, :, 0:64], in_=PF[bi][:, 0:128].rearrange("p (h e) -> p h e", h=2)))
                        heads = []
                        for hl in range(2):
                            heads.append(dict(
                                KT=lambda kt: KC[:, kt * 128:(kt + 1) * 128],
                                QT=lambda q0, n, hl=hl: QC2[hl][:, q0:q0 + n],
                                V=lambda kt, hl=hl: VC[:, kt, hl, :],
                                rk=lambda kt, qg, hl=hl: [('KC', kt // 4), ('QC', hl, qg)],
                                vk=lambda kt: [('VC', kt)],
                                cumq=cumq[hl], cqk=('cumq', hl),
                                cumk=lambda kt, hh=2 * b + hl: cumk[:, kt, hh:hh + 1]))
                        mx = mixed[b % 2]
                        mkey = ('mixed', b % 2)

                        def fin_c(qg, b=b, mx=mx, mkey=mkey):
                            for hl in range(2):
                                norm_out(2 + hl, mx[:, :, hl * 64:(hl + 1) * 64], [mkey], rden)
                            flush_chunk(mx, mkey, 6 + b, qg)
                        attend(sc, heads, 'C', 1.0, [2, 3], fin_c, stbanks=(0, 1, 4, 5, 6, 7), look=5)
                    T.barrier()
                dbg("mt%d" % l, MT[:, :, :], [MK(c, t) for c in range(8) for t in range(NT)], [128, 8, S])
                if stop_after == ('C', l):
                    done = True
            if done:
                break
            T.barrier()

            esL = ExitStack()
            combT = sbt(esL, "combT%d" % l, [64, S], BF16)
            with ExitStack() as es:
                Wo = sbt(es, "Wo", [128, 8, DM], BF16)
                for half in range(2):
                    wload(Wo[:, :, half * 512:(half + 1) * 512],
                          W["w_out"][l].rearrange("(c p) n -> p c n", p=128)[:, :, half * 512:(half + 1) * 512], ('Wo', half))
                Wpg = sbt(es, "Wpg", [128, 8, DM], BF16)
                Wpp = sbt(es, "Wpp", [128, 2, DM], BF16)
                wload(Wpp[:], W["w_ple_proj"][l].rearrange("(c p) n -> p c n", p=128), 'Wpp')
                for hf in range(2):
                    wload(Wpg[:, :, hf * 512:(hf + 1) * 512],
                          W["w_ple_gate"][l].rearrange("(c p) n -> p c n", p=128)[:, :, hf * 512:(hf + 1) * 512], ('Wpg', hf))
                for half in range(2):
                    for t in range(NT):
                        bi = nextbank((0, 1, 2, 3))
                        for c in range(8):
                            mm(PF[bi][:, :], MT[:, c, t * 128:(t + 1) * 128], Wo[:, c, half * 512:(half + 1) * 512], c == 0,
                               c == 7, [('Wo', half), MK(c, t)], [PK[bi]])
                        V('dve', 'scalar_tensor_tensor', [PK[bi], RK(t)], [RK(t)], out=R[:, t, half * 512:(half + 1) * 512],
                          in0=R[:, t, half * 512:(half + 1) * 512], scalar=ALPHA, in1=PF[bi][:, :], op0=ALU.mult, op1=ALU.add)
                layer_norm(es, "ln1_g", "ln1_b", l, "1", ntmp=1)
                dbg("h%d" % l, R[:, :, :], [RK(t) for t in range(NT)], [128, NT, DM])
                build_xt(es, "h%d" % l)
                Wr = sbt(es, "Wr", [128, 8, 36], BF16)
                rb = sbt(es, "rb", [128, 36], F32)
                lg = sbt(es, "lg", [128, 36], F32)
                em = sbt(es, "em", [128, 32], F32)
                ex = sbt(es, "ex", [128, 32], F32)
                selm = sbt(es, "selm", [128, 32], F32)
                m8 = sbt(es, "m8", [128, 8], F32)
                sm = sbt(es, "sm", [128, 8], F32)
                t4 = sbt(es, "t4", [128, 4], F32)
                oh = sbt(es, "oh", [128, 4], F32)
                chl = [sbt(es, "chl%d" % i, [128, 64], BF16) for i in range(2)]
                chf = sbt(es, "chf", [128, 32], F32)
                wload(Wr[:, :, 0:4], W["w_group"][l].rearrange("(c p) n -> p c n", p=128), 'Wr0')
                for g in range(4):
                    wload(Wr[:, :, 4 + 8 * g:12 + 8 * g], W["w_erouter"][l, g].rearrange("(c p) n -> p c n", p=128),
                          'Wr%d' % (g + 1))
                T.dma('sp', rb[:, 0:4], W["b_group"][l].partition_broadcast(128), writes=['rb0'])
                T.dma('sp', rb[:, 4:36], W["b_erouter"][l].partition_broadcast(128), writes=['rb1'])
                wk = ['Wr%d' % i for i in range(5)]
                for t in range(NT):
                    bi = nextbank((0, 1, 6, 7))
                    for c in range(8):
                        mm(PF[bi][:, 0:36], XT[:, c, t * 128:(t + 1) * 128], Wr[:, c, :], c == 0, c == 7, wk + [XK(t)],
                           [PK[bi]])
                    V('dve', 'tensor_tensor', [PK[bi], 'rb0', 'rb1'], ['lg'], out=lg[:], in0=PF[bi][:, 0:36], in1=rb[:],
                      op=ALU.add)
                    V('dve', 'reduce_max', ['lg'], ['sm0'], out=sm[:, 0:1], in_=lg[:, 0:4], axis=AX.X)
                    V('dve', 'tensor_scalar', ['lg', 'sm0'], ['oh'], out=oh[:], in0=lg[:, 0:4], scalar1=sm[:, 0:1], scalar2=None,
                      op0=ALU.is_equal)
                    V('dve', 'tensor_scalar', ['lg', 'sm0'], ['t4'], out=t4[:], in0=lg[:, 0:4], scalar1=sm[:, 0:1], scalar2=None,
                      op0=ALU.subtract)
                    V('pool', 'memset', [], ['sm1'], sm[:, 1:2], 0.0)
                    V('act', 'activation', ['t4'], ['t4', 'sm1'], out=t4[:], in_=t4[:], func=AF.Exp, accum_out=sm[:, 1:2])
                    V('dve', 'tensor_scalar', ['oh'], ['oh'], out=oh[:], in0=oh[:], scalar1=1e9, scalar2=-1e9, op0=ALU.mult,
                      op1=ALU.add)
                    V('dve', 'tensor_tensor', ['lg', 'oh'], ['em'], out=em[:].rearrange("p (g e) -> p g e", g=4),
                      in0=lg[:, 4:36].rearrange("p (g e) -> p g e", g=4), in1=oh[:].unsqueeze(2).to_broadcast([128, 4, 8]),
                      op=ALU.add)
                    V('dve', 'max', ['em'], ['m8'], out=m8[:], in_=em[:])
                    V('dve', 'tensor_scalar', ['em', 'm8'], ['selm'], out=selm[:], in0=em[:], scalar1=m8[:, 1:2], scalar2=None,
                      op0=ALU.is_ge)
                    V('dve', 'tensor_scalar', ['em', 'm8'], ['ex'], out=ex[:], in0=em[:], scalar1=m8[:, 0:1], scalar2=None,
                      op0=ALU.subtract)
                    V('act', 'activation', ['ex'], ['ex'], out=ex[:], in_=ex[:], func=AF.Exp)
                    V('dve', 'tensor_tensor', ['ex', 'selm'], ['ex'], out=ex[:], in0=ex[:], in1=selm[:], op=ALU.mult)
                    V('dve', 'reduce_sum', ['ex'], ['sm2'], out=sm[:, 2:3], in_=ex[:], axis=AX.X)
                    V('dve', 'tensor_tensor', ['sm1', 'sm2'], ['sm3'], out=sm[:, 3:4], in0=sm[:, 1:2], in1=sm[:, 2:3],
                      op=ALU.mult)
                    V('dve', 'reciprocal', ['sm3'], ['sm3'], out=sm[:, 3:4], in_=sm[:, 3:4])
                    V('dve', 'tensor_scalar', ['ex', 'sm3'], ['ex'], out=ex[:], in0=ex[:], scalar1=sm[:, 3:4], scalar2=None,
                      op0=ALU.mult)
                    ch = chl[t % 2]
                    ck = ('chl', t % 2)
                    V('dve', 'tensor_copy', ['ex'], [ck], out=ch[:, 0:32], in_=ex[:])
                    V('dve', 'tensor_copy', [ck], ['chf'], out=chf[:], in_=ch[:, 0:32])
                    V('dve', 'tensor_tensor', ['ex', 'chf', ck], [ck], out=ch[:, 32:64], in0=ex[:], in1=chf[:], op=ALU.subtract)
                    pi = 6 + (t % 2)
                    tr(pbv(pi)[0:64, 0:128], ch[:, :], [ck], [PK[pi]])
                    V('act', 'copy', [PK[pi]], [('combT', t)], out=combT[:, t * 128:(t + 1) * 128], in_=pbv(pi)[0:64, 0:128])
                dbg("combT%d" % l, combT[:, :], [('combT', t) for t in range(NT)], [64, S])
                ptm = [sbt(es, "ptm%d" % i, [128, 256], BF16) for i in range(2)]
                pTt = [sbt(es, "pTt%d" % i, [128, 2, 128], BF16) for i in range(2)]
                sg = [sbt(es, "sg%d" % i, [128, 512], F32) for i in range(2)]
                cnt = 0
                for t in range(NT):
                    pm = ptm[t % 2]
                    pk = ('ptm', t % 2)
                    wload(pm[:], p_d[l, t * 128:(t + 1) * 128, :], pk)
                    pi = 6 + (t % 2)
                    for c in range(2):
                        tr(pbv(pi)[:, c * 128:(c + 1) * 128], pm[:, c * 128:(c + 1) * 128], [pk], [PK[pi]])
                    pt = pTt[t % 2]
                    ptk = ('pTt', t % 2)
                    V('act', 'copy', [PK[pi]], [ptk], out=pt[:], in_=pbv(pi)[:, 0:256].rearrange("p (c k) -> p c k", c=2))
                    for half in range(2):
                        bg = nextbank((0, 1, 4))
                        bp = nextbank((2, 3, 5))
                        for c in range(8):
                            mm(PF[bg][:, :], XT[:, c, t * 128:(t + 1) * 128], Wpg[:, c, half * 512:(half + 1) * 512], c == 0,
                               c == 7, [('Wpg', half), XK(t)], [PK[bg]])
                        for c in range(2):
                            mm(PF[bp][:, :], pt[:, c, :], Wpp[:, c, half * 512:(half + 1) * 512], c == 0, c == 1,
                               ['Wpp', ptk], [PK[bp]])
                        s_ = sg[cnt % 2]
                        sk = ('sg', cnt % 2)
                        cnt += 1
                        V('act', 'activation', [PK[bg]], [sk], out=s_[:], in_=PF[bg][:, :], func=AF.Sigmoid)
                        V('dve', 'tensor_tensor', [sk, PK[bp]], [sk], out=s_[:], in0=s_[:], in1=PF[bp][:, :], op=ALU.mult)
                        V('dve', 'scalar_tensor_tensor', [sk, RK(t)], [RK(t)], out=R[:, t, half * 512:(half + 1) * 512],
                          in0=R[:, t, half * 512:(half + 1) * 512], scalar=ALPHA, in1=s_[:], op0=ALU.mult, op1=ALU.add)
                dbg("rple%d" % l, R[:, :, :], [RK(t) for t in range(NT)], [128, NT, DM])
            T.barrier()
            if stop_after == ('PLE', l):
                esL.close()
                break

            with ExitStack() as es:
                WG = [sbt(es, "WG%d" % i, [128, 8, 256], BF16) for i in range(2)]
                WU = [sbt(es, "WU%d" % i, [128, 8, 256], BF16) for i in range(2)]
                WD = [sbt(es, "WD%d" % i, [128, 2, DM], BF16) for i in range(4)]
                sel2 = sbt(es, "sel2", [64, 32, 128], BF16)
                cbs = [sbt(es, "cbs%d" % i, [128, 512], BF16) for i in range(2)]
                sl = [sbt(es, "sl%d" % i, [128, 512], BF16) for i in range(2)]
                tu = [sbt(es, "tu%d" % i, [128, 512], BF16) for i in range(2)]
                V('dve', 'tensor_tensor', ['ident'], ['sel2'], out=sel2[:],
                  in0=ident_b[0:64, 0:32].unsqueeze(2).to_broadcast([64, 32, 128]),
                  in1=ident_b[0:64, 32:64].unsqueeze(2).to_broadcast([64, 32, 128]), op=ALU.add)

                def load_expert(e):
                    s_ = e % 2
                    wload(WG[s_][:], W["w_gate_e"][l, e].rearrange("(c p) n -> p c n", p=128), ('WG', s_))
                    wload(WU[s_][:], W["w_up_e"][l, e].rearrange("(c p) n -> p c n", p=128), ('WU', s_))
                load_expert(0)
                cn2 = 0
                for eb in range(8):
                    for el in range(4):
                        e = eb * 4 + el
                        if e + 1 < 32:
                            load_expert(e + 1)
                        wload(WD[el][:], W["w_down_e"][l, e].rearrange("(c p) n -> p c n", p=128), ('WD', el))
                        s_ = e % 2
                        for tg in range(4):
                            cb = cbs[tg % 2]
                            cbk = ('cbs', tg % 2)
                            mm(PF[4][:, :], sel2[:, e, :], combT[:, tg * 512:(tg + 1) * 512], True, True,
                               ['sel2'] + [('combT', 4 * tg + i) for i in range(4)], [PK[4]])
                            V('act', 'copy', [PK[4]], [cbk], out=cb[:], in_=PF[4][:, :])
                            for fc in range(2):
                                bg, bu = (0, 1) if cn2 % 2 == 0 else (2, 3)
                                k2 = cn2 % 2
                                cn2 += 1
                                for c in range(8):
                                    mm(PF[bg][:, :], WG[s_][:, c, fc * 128:(fc + 1) * 128], XT[:, c, tg * 512:(tg + 1) * 512],
                                       c == 0, c == 7, [('WG', s_)] + [XK(4 * tg + i) for i in range(4)], [PK[bg]])
                                for c in range(8):
                                    mm(PF[bu][:, :], WU[s_][:, c, fc * 128:(fc + 1) * 128], XT[:, c, tg * 512:(tg + 1) * 512],
                                       c == 0, c == 7, [('WU', s_)] + [XK(4 * tg + i) for i in range(4)], [PK[bu]])
                                V('act', 'activation', [PK[bg]], [('sl', k2)], out=sl[k2][:], in_=PF[bg][:, :], func=AF.Silu)
                                V('dve', 'tensor_tensor', [PK[bu], ('sl', k2)], [('tu', k2)], out=tu[k2][:], in0=PF[bu][:, :],
                                  in1=sl[k2][:], op=ALU.mult)
                                V('pool', 'tensor_tensor', [('tu', k2), cbk], [MK(el * 2 + fc, 4 * tg + i) for i in range(4)],
                                  out=MT[:, el * 2 + fc, tg * 512:(tg + 1) * 512], in0=tu[k2][:], in1=cb[:], op=ALU.mult)
                    for t in range(NT):
                        for half in range(2):
                            bi = nextbank((5, 6, 7))
                            for sl_ in range(8):
                                mm(PF[bi][:, :], MT[:, sl_, t * 128:(t + 1) * 128],
                                   WD[sl_ // 2][:, sl_ % 2, half * 512:(half + 1) * 512], sl_ == 0, sl_ == 7,
                                   [('WD', sl_ // 2), MK(sl_, t)], [PK[bi]])
                            V('dve', 'tensor_tensor', [PK[bi], RK(t)], [RK(t)], out=R[:, t, half * 512:(half + 1) * 512],
                              in0=R[:, t, half * 512:(half + 1) * 512], in1=PF[bi][:, :], op=ALU.add)
                dbg("rmoe%d" % l, R[:, :, :], [RK(t) for t in range(NT)], [128, NT, DM])
            T.barrier()
            with ExitStack() as es:
                last = (l == nlayers - 1)

                def store(t):
                    if last:
                        T.dma('sp', out_d[t * 128:(t + 1) * 128, :], R[:, t, :], reads=[RK(t)], is_out=True)
                layer_norm(es, "ln2_g", "ln2_b", l, "2", after=store)
                if not last:
                    build_xt(es, "n%d" % l)
            T.barrier()
            esL.close()
        T.emit()
    return nc, dbg_d, T.stats


def host_consts():
    inv = (10000.0 ** (-np.arange(16, dtype=np.float32) / np.float32(16))).astype(np.float32)
    return (inv / np.float32(2 * np.pi)).astype(np.float32)


_CACHE = {}


def make_in_maps(inputs, ncores=8):
    cst = host_consts()
    maps = []
    for c in range(ncores):
        m = {"x": np.ascontiguousarray(inputs["x"][c]), "p": np.ascontiguousarray(inputs["p"][:, c]),
             "positions": np.ascontiguousarray(inputs["positions"][c]).astype(np.int32), "cst": cst}
        for n, sh in WNAMES:
            m[n] = np.ascontiguousarray(np.asarray(inputs[n], dtype=np.float32).reshape(sh))
        maps.append(m)
    return maps


def kernel(**inputs):
    inputs = {k: np.asarray(v) for k, v in inputs.items()}
    if 'nc' not in _CACHE:
        _CACHE['nc'] = build()[0]
    nc = _CACHE['nc']
    maps = make_in_maps(inputs)
    res = run_bass_kernel_spmd(nc, maps, core_ids=list(range(8)))
    return np.stack([np.asarray(r["out"], dtype=np.float32) for r in res.results], axis=0)
```

```python
import numpy as np
import concourse.bass as bass
import concourse.mybir as mybir
from concourse.bass_utils import run_bass_kernel_spmd
from contextlib import ExitStack

F32, BF16, I32 = mybir.dt.float32, mybir.dt.bfloat16, mybir.dt.int32
AF = mybir.ActivationFunctionType
ALU = mybir.AluOpType
AX = mybir.AxisListType


class Tracker:
    ENGS = ['pe', 'act', 'dve', 'pool', 'sp']

    def __init__(self, nc, es, serialize=False, same_engine_sync=True, ndma=16):
        self.nc = nc
        self.h = {'pe': nc.tensor, 'act': nc.scalar, 'dve': nc.vector, 'pool': nc.gpsimd, 'sp': nc.sync}
        self.sem = {e: es.enter_context(nc.semaphore("s_" + e)) for e in self.ENGS}
        self.ndma = ndma
        self.dsem = {q: [es.enter_context(nc.semaphore("d%s%d" % (q, i))) for i in range(ndma)] for q in ('sp', 'pool')}
        self.ops = []
        self.lastw = {}
        self.readers = {}
        self.serialize = serialize
        self.same_engine_sync = same_engine_sync
        self.out_dmas = []
        self.do_schedule = True

    def op(self, eng, fn, reads=(), writes=(), dma=False, is_out=False, cost=0.3, lat=0.0):
        idx = len(self.ops)
        deps = set()
        psr = [k for k in reads if isinstance(k, tuple) and k[0] == 'ps']
        if psr:
            reads = [k for k in reads if not (isinstance(k, tuple) and k[0] == 'ps')]
            writes = list(writes) + psr
        for k in reads:
            w = self.lastw.get(k)
            if w is not None:
                deps.add(w)
        for k in writes:
            w = self.lastw.get(k)
            if w is not None:
                deps.add(w)
            rd = self.readers.get(k)
            if rd:
                deps.update(rd)
        if self.serialize and idx > 0:
            deps.add(idx - 1)
        for k in writes:
            self.lastw[k] = idx
            self.readers[k] = []
        for k in reads:
            self.readers.setdefault(k, []).append(idx)
        deps.discard(idx)
        self.ops.append([eng, fn, deps, dma, False, None, None, cost, lat])
        if is_out:
            self.out_dmas.append(idx)
        return idx

    def dma(self, eng, out, in_, reads=(), writes=(), is_out=False, **kw):
        nbytes = 4.0
        for d in in_.shape:
            nbytes *= d
        return self.op(eng, lambda h: h.dma_start(out=out, in_=in_, **kw), reads, writes, dma=True, is_out=is_out,
                       cost=(1.0 if eng == 'pool' else 0.15), lat=2.0 + nbytes / 120e3)

    def schedule(self, window=8000):
        import heapq
        ops = self.ops
        n = len(ops)
        ndep = [len(o[2]) for o in ops]
        users = [[] for _ in range(n)]
        for i, o in enumerate(ops):
            for d in o[2]:
                users[d].append(i)
        ready_t = [0.0] * n
        fin = [0.0] * n
        heaps = {e: [] for e in self.ENGS}
        for i in range(n):
            if ndep[i] == 0:
                heapq.heappush(heaps[ops[i][0]], (i, i))
        free = {e: 0.0 for e in self.ENGS}
        order = []
        done = [False] * n
        low = 0
        while len(order) < n:
            while low < n and done[low]:
                low += 1
            best = None
            for e in self.ENGS:
                h = heaps[e]
                cands = []
                while h and len(cands) < 160:
                    c = heapq.heappop(h)
                    cands.append(c)
                pick = None
                for c in cands:
                    i = c[1]
                    if i > low + window:
                        continue
                    st = max(free[e], ready_t[i])
                    if pick is None or st < pick[0] - 1e-9:
                        pick = (st, i)
                for c in cands:
                    heapq.heappush(h, c)
                if pick is not None and (best is None or pick[0] < best[0] - 1e-9 or (abs(pick[0] - best[0]) <= 1e-9 and pick[1] < best[1])):
                    best = (pick[0], pick[1], e)
            if best is None:
                best_i = None
                for e in self.ENGS:
                    if heaps[e]:
                        i = heaps[e][0][1]
                        if best_i is None or i < best_i:
                            best_i = i
                e = ops[best_i][0]
                best = (max(free[e], ready_t[best_i]), best_i, e)
            st, i, e = best
            h = heaps[e]
            h.remove((i, i))
            heapq.heapify(h)
            o = ops[i]
            free[e] = st + o[7]
            fin[i] = st + o[7] + o[8]
            done[i] = True
            order.append(i)
            for u in users[i]:
                ndep[u] -= 1
                lat = 0.25 if ops[u][0] != e else 0.15
                if fin[i] + lat > ready_t[u]:
                    ready_t[u] = fin[i] + lat
                if ndep[u] == 0:
                    heapq.heappush(heaps[ops[u][0]], (u, u))
        self.est_time = max(fin) if fin else 0.0
        return order

    def barrier(self):
        self.op('sp', None, reads=(), writes=('__bar__',))
        self.ops[-1][1] = 'BARRIER'

    def emit(self):
        ops = self.ops
        ENGS = self.ENGS
        since = []
        bar = None
        for i, o in enumerate(ops):
            if o[1] == 'BARRIER':
                o[2] = set(since)
                if bar is not None:
                    o[2].add(bar)
                since = []
                bar = i
                continue
            if bar is not None:
                o[2].add(bar)
            since.append(i)
        for o in ops:
            for d in o[2]:
                p = ops[d]
                if (not p[3]) and p[0] == o[0] and (o[0] == 'pe' or not self.same_engine_sync) and not o[3]:
                    continue
                p[4] = True
        order = self.schedule() if self.do_schedule else list(range(len(ops)))
        cnt = {e: 0 for e in ENGS}
        seen = {e: {} for e in ENGS}
        dval = {q: [0] * self.ndma for q in ('sp', 'pool')}
        nissued = {'sp': 0, 'pool': 0}
        ndma_issued = 0
        nwait = 0
        for oi in order:
            o = ops[oi]
            eng, fn, deps, isdma, signal = o[0], o[1], o[2], o[3], o[4]
            h = self.h[eng]
            waits = {}
            for d in deps:
                p = ops[d]
                if p[3]:
                    sem, val = p[5], p[6]
                else:
                    if p[0] == eng and (eng == 'pe' or not self.same_engine_sync) and not isdma:
                        continue
                    sem, val = self.sem[p[0]], p[6]
                    if val is None:
                        raise RuntimeError("dep on non-signaling op")
                key = id(sem)
                if key not in waits or waits[key][1] < val:
                    waits[key] = (sem, val)
            for key, (sem, val) in waits.items():
                if seen[eng].get(key, 0) >= val:
                    continue
                h.wait_ge(sem, val)
                nwait += 1
                seen[eng][key] = val
            if fn == 'BARRIER':
                cnt[eng] += 1
                h.sem_inc(self.sem[eng], 1)
                o[6] = cnt[eng]
                continue
            if isdma:
                i = nissued[eng] % self.ndma
                nissued[eng] += 1
                ndma_issued += 1
                sem = self.dsem[eng][i]
                dv = dval[eng]
                if dv[i] > 0 and seen[eng].get(id(sem), 0) < dv[i]:
                    h.wait_ge(sem, dv[i])
                    seen[eng][id(sem)] = dv[i]
                ins = fn(h)
                ins.then_inc(sem, 16)
                dv[i] += 16
                o[5], o[6] = sem, dv[i]
            else:
                ins = fn(h)
                if signal:
                    cnt[eng] += 1
                    ins.then_inc(self.sem[eng], 1)
                    o[6] = cnt[eng]
        h = self.h['sp']
        for d in self.out_dmas:
            p = ops[d]
            h.wait_ge(p[5], p[6])
        self.stats = dict(nops=len(ops), nwait=nwait, cnt=cnt, ndma=ndma_issued)


S = 2048
DM = 1024
NT = 16
DEPTH = 2
ALPHA = float((2 * DEPTH) ** 0.25)
C_QA, C_KA, C_VA, C_CQ, C_CKV, C_KR, C_QC, C_KC, C_VC, C_FC = 0, 384, 768, 1152, 1408, 1536, 1568, 1824, 2080, 2336
SLOPES = [float(2.0 ** (-8.0 * (i + 1) / 6)) for i in range(6)]

WNAMES = [("w_in", [DEPTH, 1024, 2340]), ("w_uq", [DEPTH, 256, 576]), ("w_ukv", [DEPTH, 128, 768]),
          ("g_cq", [DEPTH, 256]), ("g_ckv", [DEPTH, 128]), ("lam_q1", [DEPTH, 32]), ("lam_k1", [DEPTH, 32]),
          ("lam_q2", [DEPTH, 32]), ("lam_k2", [DEPTH, 32]), ("g_diff", [DEPTH, 64]), ("b_forget", [DEPTH, 4]),
          ("w_out", [DEPTH, 1024, 1024]), ("ln1_g", [DEPTH, 1024]), ("ln1_b", [DEPTH, 1024]),
          ("w_group", [DEPTH, 1024, 4]), ("b_group", [DEPTH, 4]), ("w_erouter", [DEPTH, 4, 1024, 8]),
          ("b_erouter", [DEPTH, 32]), ("w_gate_e", [DEPTH, 32, 1024, 256]), ("w_up_e", [DEPTH, 32, 1024, 256]),
          ("w_down_e", [DEPTH, 32, 256, 1024]), ("w_ple_gate", [DEPTH, 1024, 1024]),
          ("w_ple_proj", [DEPTH, 256, 1024]), ("ln2_g", [DEPTH, 1024]), ("ln2_b", [DEPTH, 1024])]


def build(nlayers=DEPTH, serialize=False, same_engine_sync=True, dbg_names=(), stop_after=None):
    nc = bass.Bass("TRN2", target_bir_lowering=False)
    x_d = nc.dram_tensor("x", [S, DM], F32, kind="ExternalInput").ap()
    p_d = nc.dram_tensor("p", [DEPTH, S, 256], F32, kind="ExternalInput").ap()
    pos_d = nc.dram_tensor("positions", [S], I32, kind="ExternalInput").ap()
    cst_d = nc.dram_tensor("cst", [16], F32, kind="ExternalInput").ap()
    W = {n: nc.dram_tensor(n, sh, F32, kind="ExternalInput").ap() for n, sh in WNAMES}
    out_d = nc.dram_tensor("out", [S, DM], F32, kind="ExternalOutput").ap()
    dbg_d = {}

    top = ExitStack()
    with top:
        T = Tracker(nc, top, serialize=serialize, same_engine_sync=same_engine_sync)

        uid = [0]

        def sbt(es, name, shape, dt):
            uid[0] += 1
            return es.enter_context(nc.sbuf_tensor("%s_%d" % (name, uid[0]), shape, dt))

        def dbg(name, ap, reads, shape):
            if name not in dbg_names:
                return
            d = nc.dram_tensor("dbg_" + name, list(shape), ap.dtype, kind="ExternalOutput").ap()
            dbg_d[name] = d
            T.dma('sp', d, ap, reads=reads, is_out=True)

        R = sbt(top, "R", [128, NT, DM], F32)
        XT = sbt(top, "XT", [128, 8, S], BF16)
        MT = sbt(top, "MT", [128, 8, S], BF16)
        ident_f = sbt(top, "ident_f", [128, 128], F32)
        ident_b = sbt(top, "ident_b", [128, 128], BF16)
        posk = sbt(top, "posk", [128, NT], F32)
        negposk = sbt(top, "negposk", [128, NT], F32)
        cc_t = sbt(top, "cc_t", [128, NT, 32], F32)
        ss_t = sbt(top, "ss_t", [128, NT, 32], F32)
        posmask = sbt(top, "posmask", [128, 128], F32)
        one1 = sbt(top, "one1", [128, 1], F32)
        slp6 = sbt(top, "slp6", [128, 6], F32)
        PF = [top.enter_context(nc.psum_tensor("pf%d" % i, [128, 512], F32)) for i in range(8)]
        PK = [('ps', i) for i in range(8)]

        def pbv(i):
            return PF[i][:].bitcast(BF16)

        RK = lambda t: ('R', t)
        XK = lambda t: ('XT', t)
        MK = lambda c, t: ('MT', c, t)

        def fsize(ap):
            n = 1
            for d in ap.shape[1:]:
                n *= d
            return n

        def V(eng, method, reads, writes, *a, **kw):
            o = kw.get('out', a[0] if a else None)
            n = fsize(o) if o is not None else 64
            if eng == 'act':
                c = 0.3 + n / 1400.0
            elif eng == 'dve':
                c = 0.22 + n / 960.0
            else:
                c = 0.35 + n / 420.0
            T.op(eng, lambda h: getattr(h, method)(*a, **kw), reads, writes, cost=c)

        def mm(out, lhsT, rhs, start, stop, reads, writes, **kw):
            n = max(fsize(out), 64)
            kk = lhsT.shape[0]
            f32 = 4.0 if lhsT.dtype == F32 else 1.0
            c = 0.012 + (n / 2400.0) * (1.0 + 1.4 * (128 - kk) / 96.0 if kk < 128 else 1.0) * f32 + (0.03 if n < 256 else 0.0)
            T.op('pe', lambda h: h.matmul(out, lhsT, rhs, start=start, stop=stop, **kw), reads, writes, cost=c)

        def tr(out, in_, reads, writes):
            ident = ident_b if in_.dtype == BF16 else ident_f
            T.op('pe', lambda h: h.transpose(out=out, in_=in_, identity=ident[:]), list(reads) + ['ident'], writes, cost=0.09)

        with ExitStack() as es0:
            posk_i = sbt(es0, "posk_i", [128, NT], I32)
            cst = sbt(es0, "cst_sb", [128, 16], F32)
            ang = sbt(es0, "ang", [128, NT, 16], F32)
            angi = sbt(es0, "angi", [128, NT, 16], I32)
            angf = sbt(es0, "angf", [128, NT, 16], F32)
            angg = sbt(es0, "angg", [128, NT, 16], F32)
            T.dma('sp', posk_i[:], pos_d.rearrange("(t p) -> p t", p=128), writes=['posk_i'],
                  allow_slow_non_contiguous=True)
            T.dma('sp', cst[:], cst_d.partition_broadcast(128), writes=['cst'])
            V('dve', 'tensor_copy', ['posk_i'], ['posk'], out=posk[:], in_=posk_i[:])
            V('dve', 'tensor_scalar', ['posk'], ['negposk'], out=negposk[:], in0=posk[:], scalar1=-1.0, scalar2=None, op0=ALU.mult)
            V('pool', 'memset', [], ['ident'], ident_f[:], 1.0)
            V('pool', 'memset', [], ['one1'], one1[:], 1.0)
            for i in range(6):
                V('pool', 'memset', ['slp6'] if i else [], ['slp6'], slp6[:, i:i + 1], SLOPES[i] * float(np.sqrt(32.0)))
            V('pool', 'affine_select', ['ident'], ['ident'], out=ident_f[:], in_=ident_f[:], pattern=[[-1, 128]],
              compare_op=ALU.is_equal, fill=0.0, base=0, channel_multiplier=1)
            V('dve', 'tensor_copy', ['ident'], ['ident'], out=ident_b[:], in_=ident_f[:])
            V('pool', 'memset', [], ['posmask'], posmask[:], 0.0)
            V('pool', 'affine_select', ['posmask'], ['posmask'], out=posmask[:], in_=posmask[:], pattern=[[1, 128]],
              compare_op=ALU.is_ge, fill=30000.0, base=0, channel_multiplier=-1)

            def table(dst_lo, dst_hi, phase, sign_lo, sign_hi):
                V('dve', 'tensor_tensor', ['posk', 'cst'], ['ang'], out=ang[:],
                  in0=posk[:].unsqueeze(2).to_broadcast([128, NT, 16]),
                  in1=cst[:, 0:16].unsqueeze(1).to_broadcast([128, NT, 16]), op=ALU.mult)
                if phase != 0.0:
                    V('dve', 'tensor_scalar', ['ang'], ['ang'], out=ang[:], in0=ang[:], scalar1=phase, scalar2=None,
                      op0=ALU.add)
                V('dve', 'tensor_copy', ['ang'], ['angi'], out=angi[:], in_=ang[:])
                V('dve', 'tensor_copy', ['angi'], ['angf'], out=angf[:], in_=angi[:])
                V('dve', 'tensor_tensor', ['ang', 'angf'], ['ang'], out=ang[:], in0=ang[:], in1=angf[:], op=ALU.subtract)
                V('dve', 'tensor_scalar', ['ang'], ['angg'], out=angg[:], in0=ang[:], scalar1=0.5, scalar2=None,
                  op0=ALU.is_gt)
                V('dve', 'tensor_tensor', ['ang', 'angg'], ['ang'], out=ang[:], in0=ang[:], in1=angg[:], op=ALU.subtract)
                V('dve', 'tensor_scalar', ['ang'], ['angg'], out=angg[:], in0=ang[:], scalar1=-0.5, scalar2=None,
                  op0=ALU.is_lt)
                V('dve', 'tensor_tensor', ['ang', 'angg'], ['ang'], out=ang[:], in0=ang[:], in1=angg[:], op=ALU.add)
                V('act', 'activation', ['ang'], ['angf'], out=angf[:], in_=ang[:], func=AF.Sin, scale=float(2 * np.pi))
                V('dve', 'tensor_scalar', ['angf'], ['tab'], out=dst_lo, in0=angf[:], scalar1=sign_lo, scalar2=None,
                  op0=ALU.mult)
                V('dve', 'tensor_scalar', ['angf'], ['tab'], out=dst_hi, in0=angf[:], scalar1=sign_hi, scalar2=None,
                  op0=ALU.mult)
            table(cc_t[:, :, 0:16], cc_t[:, :, 16:32], 0.25, 1.0, 1.0)
            table(ss_t[:, :, 0:16], ss_t[:, :, 16:32], 0.0, -1.0, 1.0)
            T.barrier()
        dbg("cc", cc_t[:, :, :], ['tab'], [128, NT, 32])
        dbg("ss", ss_t[:, :, :], ['tab'], [128, NT, 32])

        for t in range(NT):
            T.dma('sp', R[:, t, :], x_d[t * 128:(t + 1) * 128, :], writes=[RK(t)])

        def build_xt(es, tag):
            xb = [sbt(es, "xb%s%d" % (tag, i), [128, DM], BF16) for i in range(2)]
            for t in range(NT):
                b = xb[t % 2]
                bk = ('xb', t % 2)
                V('act', 'copy', [RK(t)], [bk], out=b[:], in_=R[:, t, :])
                pi = 4 + (t % 4)
                for c in range(8):
                    tr(pbv(pi)[:, c * 128:(c + 1) * 128], b[:, c * 128:(c + 1) * 128], [bk], [PK[pi]])
                V('dve', 'tensor_copy', [PK[pi]], [XK(t)], out=XT[:, :, t * 128:(t + 1) * 128],
                  in_=pbv(pi).rearrange("p (c k) -> p c k", c=8))

        def wload(dst, src, key):
            T.dma('pool', dst, src, writes=[key])

        def win_cols(l, c0, n):
            return W["w_in"][l].rearrange("(c p) n -> p c n", p=128)[:, :, c0:c0 + n]

        bank_rr = [0]

        def nextbank(choices):
            bank_rr[0] += 1
            return choices[bank_rr[0] % len(choices)]

        def proj_fm(Wt, wkey, col0, M, evac, banks=(0, 1, 6, 7)):
            for tg in range(4):
                bi = nextbank(banks)
                for c in range(8):
                    mm(PF[bi][0:M, :], Wt[:, c, col0:col0 + M], XT[:, c, tg * 512:(tg + 1) * 512], c == 0, c == 7,
                       [wkey] + [XK(4 * tg + i) for i in range(4)], [PK[bi]])
                evac(tg, bi)

        def proj_tm(Wt, wkey, col0, N, evac, banks=(0, 1, 6, 7)):
            for t in range(NT):
                bi = nextbank(banks)
                for c in range(8):
                    mm(PF[bi][:, 0:N], XT[:, c, t * 128:(t + 1) * 128], Wt[:, c, col0:col0 + N], c == 0, c == 7,
                       [wkey, XK(t)], [PK[bi]])
                evac(t, bi)

        def attend(sc, heads, mode, scale, obanks, finalize, stbanks=(0, 1, 7), look=2):
            assert look + 1 <= len(sc['PT']) and look + 1 <= len(stbanks) + 0
            from collections import deque
            PT, TB, DT = sc['PT'], sc['TB'], sc['DT']
            posq = sc.get('posq')
            cnt = sc['cnt']
            pending = deque()

            def retire():
                x = pending.popleft()
                x()

            for qg in range(4):
                for kt in range(4 * qg + 4):
                    q0 = max(qg * 512, kt * 128)
                    n = (qg + 1) * 512 - q0
                    diag = kt >= 4 * qg
                    dk = None
                    di = 0
                    if mode == 'A':
                        di = cnt[2] % 3
                        cnt[2] += 1
                        dk = ('DT', di)
                        V('pool', 'tensor_tensor', ['posq', 'posk'], [dk], out=DT[di][:, 0:n], in0=posq[:, q0:q0 + n],
                          in1=posk[:, kt:kt + 1].to_broadcast([128, n]), op=ALU.subtract)
                        V('dve', 'scalar_tensor_tensor', [dk], [dk], out=DT[di][:, 0:n], in0=DT[di][:, 0:n], scalar=-1.0,
                          in1=DT[di][:, 0:n], op0=ALU.mult, op1=ALU.min)
                    for hi, hd in enumerate(heads):
                        si = stbanks[cnt[0] % len(stbanks)]
                        cnt[0] += 1
                        pi = cnt[1] % 6
                        ti = cnt[1] % len(TB)
                        cnt[1] += 1
                        ptk = ('PT', pi)
                        tbk = ('TB', ti)
                        mm(PF[si][:, 0:n], hd['KT'](kt), hd['QT'](q0, n), True, mode != 'A', hd['rk'](kt, qg), [PK[si]],
                           **hd.get('kw', {}))
                        if mode == 'A':
                            bk = ('BH', di, hi // 2)
                            if hi % 2 == 0:
                                V('dve', 'tensor_scalar', [dk], [bk], out=sc['BH'][di][hi // 2][:, 0:n], in0=DT[di][:, 0:n],
                                  scalar1=hd['slope'], scalar2=None, op0=ALU.mult)
                            mm(PF[si][:, 0:n], ident_b[:, :], sc['BH'][di][hi // 2][:, 0:n], False, True, [bk, 'ident'], [PK[si]])
                            V('act', 'activation', [PK[si]], [ptk], out=PT[pi][:, 0:n], in_=PF[si][:, 0:n], func=AF.Exp,
                              scale=scale)
                        elif mode == 'B':
                            V('act', 'activation', [PK[si]], [ptk], out=PT[pi][:, 0:n], in_=PF[si][:, 0:n], func=AF.Exp,
                              scale=scale)
                        else:
                            V('dve', 'scalar_tensor_tensor', [hd['cqk'], 'cumk', PK[si]], [tbk], out=TB[ti][:, 0:n],
                              in0=hd['cumq'][:, q0:q0 + n], scalar=hd['cumk'](kt), in1=PF[si][:, 0:n],
                              op0=ALU.subtract, op1=ALU.subtract)
                            if diag:
                                V('dve', 'tensor_tensor', [tbk, 'posmask'], [tbk], out=TB[ti][:, 0:128],
                                  in0=TB[ti][:, 0:128], in1=posmask[:], op=ALU.add)
                            V('act', 'activation', [tbk], [ptk], out=PT[pi][:, 0:n], in_=TB[ti][:, 0:n], func=AF.Exp,
                              scale=-1.0)
                        if diag and mode != 'C':
                            V('pool', 'memset', [ptk], [ptk], PT[pi][64:128, 0:64], 0.0)
                        ob = obanks[hi]

                        def pv(kt=kt, q0=q0, n=n, qg=qg, pi=pi, ptk=ptk, hd=hd, ob=ob):
                            for j in range(n // 128):
                                qt = q0 // 128 + j
                                jj = qt - 4 * qg
                                mm(PF[ob][:, jj * 128:jj * 128 + 65], PT[pi][:, j * 128:(j + 1) * 128], hd['V'](kt),
                                   kt == 0 and j == 0, kt == qt, [ptk] + hd['vk'](kt), [PK[ob]], skip_group_check=True)
                        pending.append(pv)
                        while len(pending) > look:
                            retire()
                pending.append(lambda qg=qg: finalize(qg))
            while pending:
                retire()

        def norm_out(ob, dst, dkeys, rden):
            Ov = PF[ob][:, :].rearrange("p (j e) -> p j e", e=128)
            V('dve', 'reciprocal', [PK[ob]], ['rden'], out=rden[:], in_=Ov[:, :, 64])
            V('dve', 'tensor_tensor', [PK[ob], 'rden'], dkeys, out=dst, in0=Ov[:, :, 0:64],
              in1=rden[:].unsqueeze(2).to_broadcast([128, 4, 64]), op=ALU.mult)

        def flush_chunk(mixed, mkey, chunk, qg, pi=6):
            for j in range(4):
                tr(pbv(pi)[:, j * 128:(j + 1) * 128], mixed[:, j, :], [mkey], [PK[pi]])
            V('act', 'copy', [PK[pi]], [MK(chunk, 4 * qg + i) for i in range(4)],
              out=MT[:, chunk, qg * 512:(qg + 1) * 512], in_=pbv(pi)[:, 0:512])

        def layer_norm(es, gname, bname, l, tag, after=None, ntmp=2):
            g_b = sbt(es, "lng" + tag, [128, DM], F32)
            b_b = sbt(es, "lnb" + tag, [128, DM], F32)
            st = sbt(es, "lnst" + tag, [128, 2, 6], F32)
            ag = sbt(es, "lnag" + tag, [128, 2], F32)
            rs = sbt(es, "lnrs" + tag, [128, 1], F32)
            nb_ = sbt(es, "lnnb" + tag, [128, 1], F32)
            tmp = [sbt(es, "lntmp%s%d" % (tag, i), [128, DM], F32) for i in range(ntmp)]
            T.dma('sp', g_b[:], W[gname][l].partition_broadcast(128), writes=['lng'])
            T.dma('sp', b_b[:], W[bname][l].partition_broadcast(128), writes=['lnb'])
            for t in range(NT):
                tk = ('lntmp', t % ntmp)
                tm = tmp[t % ntmp]
                V('dve', 'bn_stats', [RK(t)], ['lnst'], out=st[:, 0, :], in_=R[:, t, 0:512])
                V('dve', 'bn_stats', [RK(t), 'lnst'], ['lnst'], out=st[:, 1, :], in_=R[:, t, 512:1024])
                V('dve', 'bn_aggr', ['lnst'], ['lnag'], out=ag[:], in_=st[:])
                V('act', 'activation', ['lnag'], ['lnrs'], out=rs[:], in_=ag[:, 1:2], func=AF.Sqrt, bias=1e-5, scale=1.0)
                V('dve', 'reciprocal', ['lnrs'], ['lnrs'], out=rs[:], in_=rs[:])
                V('dve', 'scalar_tensor_tensor', ['lnag', 'lnrs'], ['lnnb'], out=nb_[:], in0=ag[:, 0:1], scalar=-1.0, in1=rs[:],
                  op0=ALU.mult, op1=ALU.mult)
                V('act', 'activation', [RK(t), 'lnnb', 'lnrs'], [tk], out=tm[:], in_=R[:, t, :], func=AF.Identity,
                  scale=rs[:, 0:1], bias=nb_[:, 0:1])
                V('dve', 'tensor_tensor', [tk, 'lng'], [tk], out=tm[:], in0=tm[:], in1=g_b[:], op=ALU.mult)
                V('pool', 'tensor_tensor', [tk, 'lnb'], [RK(t)], out=R[:, t, :], in0=tm[:], in1=b_b[:], op=ALU.add)
                if after is not None:
                    after(t)

        done = False
        for l in range(nlayers):
            if done:
                break
            lam_init = 0.8 - 0.6 * float(np.exp(-0.3 * l))
            with ExitStack() as esA:
                if l == 0:
                    with ExitStack() as esx:
                        build_xt(esx, "a%d" % l)
                        T.barrier()
                dbg("xt%d" % l, XT[:, :, :], [XK(t) for t in range(NT)], [128, 8, S])
                sc = dict(PT=[sbt(esA, "PT%d" % i, [128, 512], BF16) for i in range(6)], TB=None, DT=None, cnt=[0, 0, 0])
                rden = sbt(esA, "rden", [128, 4], F32)
                mixed = [sbt(esA, "mixed%d" % i, [128, 4, 128], BF16) for i in range(2)]
                lamt = sbt(esA, "lamt", [128, 4, 32], F32)
                lamp = sbt(esA, "lamp", [128, 2, 32], F32)
                lams = sbt(esA, "lams", [128, 2], F32)
                neglam = sbt(esA, "neglam", [128, 1], F32)
                gdiff = sbt(esA, "gdiff", [128, 64], F32)
                for i, nm in enumerate(["lam_q1", "lam_k1", "lam_q2", "lam_k2"]):
                    T.dma('sp', lamt[:, i, :], W[nm][l].partition_broadcast(128), writes=['lamt%d' % i])
                T.dma('sp', gdiff[:], W["g_diff"][l].partition_broadcast(128), writes=['gdiff'])
                V('dve', 'tensor_tensor', ['lamt0', 'lamt1'], ['lamp'], out=lamp[:, 0, :], in0=lamt[:, 0, :], in1=lamt[:, 1, :],
                  op=ALU.mult)
                V('dve', 'tensor_tensor', ['lamt2', 'lamt3', 'lamp'], ['lamp'], out=lamp[:, 1, :], in0=lamt[:, 2, :],
                  in1=lamt[:, 3, :], op=ALU.mult)
                V('dve', 'reduce_sum', ['lamp'], ['lams'], out=lams[:], in_=lamp[:], axis=AX.X)
                V('act', 'activation', ['lams'], ['lams'], out=lams[:], in_=lams[:], func=AF.Exp)
                V('dve', 'tensor_tensor', ['lams'], ['neglam'], out=neglam[:], in0=lams[:, 1:2], in1=lams[:, 0:1],
                  op=ALU.subtract)
                V('dve', 'tensor_scalar', ['neglam'], ['neglam'], out=neglam[:], in0=neglam[:], scalar1=-lam_init,
                  scalar2=None, op0=ALU.add)
                V('dve', 'tensor_scalar', ['gdiff'], ['gdiff'], out=gdiff[:], in0=gdiff[:], scalar1=1.0 - lam_init,
                  scalar2=None, op0=ALU.mult)

                with ExitStack() as es:
                    sc['TB'] = [None]
                    posq = sbt(es, "posq", [128, S], F32)
                    with ExitStack() as esp:
                        posq_i = sbt(esp, "posq_i", [128, S], I32)
                        T.dma('sp', posq_i[:], pos_d.partition_broadcast(128), writes=['posq_i'])
                        V('dve', 'tensor_copy', ['posq_i'], ['posq'], out=posq[:], in_=posq_i[:])
                        T.barrier()
                    sc['posq'] = posq
                    sc['DT'] = [sbt(es, "DT%d" % i, [128, 512], F32) for i in range(3)]
                    sc['BH'] = [[sbt(es, "BH%d_%d" % (i, j), [128, 512], BF16) for j in range(2)] for i in range(3)]
                    WA = sbt(es, "WA", [128, 8, 384], BF16)
                    QA4 = [sbt(es, "QA%d" % i, [128, S], BF16) for i in range(4)]
                    for i in range(4):
                        if i % 2 == 0:
                            V('act', 'memzero', [], [('QA', i, tg) for tg in range(4)], QA4[i][:])
                        else:
                            V('dve', 'memset', [], [('QA', i, tg) for tg in range(4)], QA4[i][:], 0.0)
                    KA = sbt(es, "KA", [128, S], BF16)
                    VA = sbt(es, "VA", [128, NT, 2, 65], BF16)
                    oA = [sbt(es, "oA%d" % i, [128, 4, 64], F32) for i in range(4)]
                    dA = sbt(es, "dA", [128, 4, 64], F32)
                    sqA = sbt(es, "sqA", [128, 4, 64], F32)
                    ssA = sbt(es, "ssA", [128, 4], F32)
                    V('pool', 'memset', [], [('VA', t) for t in range(NT)], VA[:, :, :, 64:65], 1.0)
                    scale_a = 1.0 / float(np.sqrt(32.0))
                    for b in range(3):
                        for i, c0 in enumerate([C_QA, C_KA, C_VA]):
                            wload(WA[:, :, i * 128:(i + 1) * 128], win_cols(l, c0 + b * 128, 128), 'WA%d' % i)
                        def evac_qa(tg, bi):
                            for i in range(4):
                                V('act' if i % 2 == 0 else 'dve', 'copy' if i % 2 == 0 else 'tensor_copy', [PK[bi]], [('QA', i, tg)],
                                  out=QA4[i][32 * i:32 * i + 32, tg * 512:(tg + 1) * 512], in_=PF[bi][32 * i:32 * i + 32, :])
                        proj_fm(WA, 'WA0', 0, 128, evac_qa)
                        proj_fm(WA, 'WA1', 128, 128, lambda tg, bi: V('act', 'copy', [PK[bi]], [('KA', tg)],
                                out=KA[:, tg * 512:(tg + 1) * 512], in_=PF[bi][:, :]))
                        proj_tm(WA, 'WA2', 256, 128, lambda t, bi: V('dve', 'tensor_copy', [PK[bi]], [('VA', t)],
                                out=VA[:, t, :, 0:64], in_=PF[bi][:, 0:128].rearrange("p (h e) -> p h e", h=2)))
                        if b == 0:
                            dbg("va%d" % l, VA[:, :, :, :], [('VA', i) for i in range(NT)], [128, NT, 2, 65])
                        heads = []
                        for hl in range(2):
                            for m in range(2):
                                p0 = hl * 64 + m * 32
                                heads.append(dict(
                                    KT=lambda kt: KA[:, kt * 128:(kt + 1) * 128],
                                    QT=lambda q0, n, i=p0 // 32: QA4[i][:, q0:q0 + n],
                                    V=lambda kt, hl=hl: VA[:, kt, hl, :],
                                    rk=lambda kt, qg, i=p0 // 32: [('KA', kt // 4), ('QA', i, qg)],
                                    vk=lambda kt: [('VA', kt)],
                                    slope=SLOPES[2 * b + hl] / scale_a, h=2 * b + hl))
                        mx = mixed[b % 2]
                        mkey = ('mixed', b % 2)

                        def fin_a(qg, b=b, mx=mx, mkey=mkey):
                            for hi in range(4):
                                norm_out(2 + hi, oA[hi][:], [('oA', hi)], rden)
                            for hl in range(2):
                                o1, o2 = oA[2 * hl], oA[2 * hl + 1]
                                V('dve', 'scalar_tensor_tensor', [('oA', 2 * hl), ('oA', 2 * hl + 1), 'neglam'], ['dA'],
                                  out=dA[:], in0=o2[:], scalar=neglam[:, 0:1], in1=o1[:], op0=ALU.mult, op1=ALU.add)
                                V('pool', 'tensor_tensor', ['dA'], ['sqA'], out=sqA[:], in0=dA[:], in1=dA[:], op=ALU.mult)
                                V('dve', 'reduce_sum', ['sqA'], ['ssA'], out=ssA[:], in_=sqA[:], axis=AX.X)
                                V('act', 'activation', ['ssA'], ['ssA'], out=ssA[:], in_=ssA[:], func=AF.Sqrt,
                                  scale=1.0 / 64.0, bias=1e-6)
                                V('dve', 'reciprocal', ['ssA'], ['ssA'], out=ssA[:], in_=ssA[:])
                                V('dve', 'tensor_tensor', ['dA', 'ssA'], ['sqA'], out=sqA[:], in0=dA[:],
                                  in1=ssA[:].unsqueeze(2).to_broadcast([128, 4, 64]), op=ALU.mult)
                                V('pool', 'tensor_tensor', ['sqA', 'gdiff'], [mkey], out=mx[:, :, hl * 64:(hl + 1) * 64],
                                  in0=sqA[:], in1=gdiff[:].unsqueeze(1).to_broadcast([128, 4, 64]), op=ALU.mult)
                            flush_chunk(mx, mkey, b, qg)
                        attend(sc, heads, 'A', scale_a, [2, 3, 4, 5], fin_a, stbanks=(0, 1, 6, 7), look=3)
                    T.barrier()
                dbg("mtA%d" % l, MT[:, 0:3, :], [MK(c, t) for c in range(3) for t in range(NT)], [128, 3, S])
                if stop_after == ('A', l):
                    done = True

                if not done:
                  with ExitStack() as es:
                    WB = sbt(es, "WB", [128, 8, 416], BF16)
                    Wuq = sbt(es, "Wuq", [128, 2, 576], BF16)
                    Wukv = sbt(es, "Wukv", [128, 768], BF16)
                    gq_b = sbt(es, "gq_b", [128, 256], F32)
                    gkv_b = sbt(es, "gkv_b", [128, 128], F32)
                    cqT = sbt(es, "cqT", [128, 2, S], BF16)
                    ckvT = sbt(es, "ckvT", [128, S], BF16)
                    QBs = [sbt(es, "QB%d" % i, [128, 2, S], BF16) for i in range(2)]
                    KBs = [sbt(es, "KB%d" % i, [128, 2, S], BF16) for i in range(2)]
                    VBs = [sbt(es, "VB%d" % i, [128, NT, 2, 65], BF16) for i in range(1)] * 2
                    for i in range(2):
                        V('act', 'memzero', [], [('QB', i, j) for j in range(4)], QBs[i][96:128, :, :])
                        V('dve', 'memset', [], [('KB', i, j) for j in range(4)], KBs[i][96:128, :, :], 0.0)
                        if i == 0:
                            V('pool', 'memset', [], [('VB', 0, t) for t in range(NT)], VBs[0][:, :, :, 64:65], 1.0)
                    stats4 = [sbt(es, "statB%d" % i, [128, 4], F32) for i in range(4)]
                    junks = [sbt(es, "junkB%d" % i, [128, 256], F32) for i in range(1)] * 2
                    cqn = [sbt(es, "cqn%d" % i, [128, 384], BF16) for i in range(2)] * 2
                    krs = [sbt(es, "krs%d" % i, [128, 96], BF16) for i in range(4)]
                    rt1s = [sbt(es, "rt1%d" % i, [128, 2, 32], F32) for i in range(2)]
                    rt2s = [sbt(es, "rt2%d" % i, [128, 2, 32], F32) for i in range(2)]
                    qst = [sbt(es, "qst%d" % i, [128, 2, 96], BF16) for i in range(2)]
                    wload(WB[:], win_cols(l, C_CQ, 416), 'WB')
                    wload(Wuq[:], W["w_uq"][l].rearrange("(c p) n -> p c n", p=128), 'Wuq')
                    wload(Wukv[:], W["w_ukv"][l], 'Wukv')
                    T.dma('sp', gq_b[:], W["g_cq"][l].partition_broadcast(128), writes=['gq_b'])
                    T.dma('sp', gkv_b[:], W["g_ckv"][l].partition_broadcast(128), writes=['gkv_b'])
                    for i in range(4):
                        V('pool', 'memset', [], [('krs', i)], krs[i][:], 0.0)

                    def evac_b1(t, bi):
                        pb = PF[bi]
                        r4 = t % 4
                        stat = stats4[r4]
                        sk_ = lambda i: ('statB', r4, i)
                        jk = ('junkB', 0)
                        jn = junks[t % 2]
                        V('pool', 'memset', [], [sk_(0), sk_(1)], stat[:, 0:2], 0.0)
                        V('act', 'activation', [PK[bi]], [jk, sk_(0)], out=jn[:, 0:256], in_=pb[:, 0:256],
                          func=AF.Square, accum_out=stat[:, 0:1])
                        V('act', 'activation', [PK[bi], jk], [jk, sk_(1)], out=jn[:, 0:128], in_=pb[:, 256:384],
                          func=AF.Square, accum_out=stat[:, 1:2])
                        V('act', 'activation', [sk_(0)], [sk_(2)], out=stat[:, 2:3], in_=stat[:, 0:1], func=AF.Sqrt,
                          scale=1.0 / 256.0, bias=1e-6)
                        V('act', 'activation', [sk_(1)], [sk_(3)], out=stat[:, 3:4], in_=stat[:, 1:2], func=AF.Sqrt,
                          scale=1.0 / 128.0, bias=1e-6)
                        V('dve', 'reciprocal', [sk_(2), sk_(3)], [sk_(4)], out=stat[:, 2:4], in_=stat[:, 2:4])
                        cn = cqn[r4]
                        ck = ('cqn', r4 % 2)
                        V('dve', 'scalar_tensor_tensor', [PK[bi], sk_(4), 'gq_b'], [ck], out=cn[:, 0:256], in0=pb[:, 0:256],
                          scalar=stat[:, 2:3], in1=gq_b[:], op0=ALU.mult, op1=ALU.mult)
                        V('dve', 'scalar_tensor_tensor', [PK[bi], sk_(4), 'gkv_b', ck], [ck], out=cn[:, 256:384],
                          in0=pb[:, 256:384], scalar=stat[:, 3:4], in1=gkv_b[:], op0=ALU.mult, op1=ALU.mult)
                        ks = krs[r4]
                        kk = ('krs', r4)
                        r1, r2 = rt1s[t % 2], rt2s[t % 2]
                        r1k, r2k = ('rt1', t % 2), ('rt2', t % 2)
                        V('dve', 'tensor_tensor', [PK[bi], 'tab'], [r1k], out=r1[:, 0, :], in0=pb[:, 384:416],
                          in1=cc_t[:, t, :], op=ALU.mult)
                        V('dve', 'tensor_tensor', [PK[bi], 'tab'], [r2k], out=r2[:, 0, 0:16], in0=pb[:, 400:416],
                          in1=ss_t[:, t, 0:16], op=ALU.mult)
                        V('dve', 'tensor_tensor', [PK[bi], 'tab', r2k], [r2k], out=r2[:, 0, 16:32], in0=pb[:, 384:400],
                          in1=ss_t[:, t, 16:32], op=ALU.mult)
                        V('pool', 'tensor_tensor', [r1k, r2k, kk], [kk], out=ks[:, 64:96], in0=r1[:, 0, :],
                          in1=r2[:, 0, :], op=ALU.add)
                        pi = 4 + (t % 4)
                        for c in range(3):
                            tr(pbv(pi)[:, c * 128:(c + 1) * 128], cn[:, c * 128:(c + 1) * 128], [ck], [PK[pi]])
                        tr(pbv(pi)[0:96, 384:512], ks[:, :], [kk], [PK[pi]])
                        V('dve', 'tensor_copy', [PK[pi]], [('cqT', t)], out=cqT[:, :, t * 128:(t + 1) * 128],
                          in_=pbv(pi)[:, 0:256].rearrange("p (c k) -> p c k", c=2))
                        V('dve', 'tensor_copy', [PK[pi]], [('ckvT', t)], out=ckvT[:, t * 128:(t + 1) * 128], in_=pbv(pi)[:, 256:384])
                        for i in range(2):
                            V('dve', 'tensor_copy', [PK[pi]], [('KBr', i, t)], out=KBs[i][64:96, :, t * 128:(t + 1) * 128],
                              in_=pbv(pi)[64:96, 384:512].unsqueeze(1).to_broadcast([32, 2, 128]))
                    proj_tm(WB, 'WB', 0, 416, evac_b1, banks=(0, 1, 2, 3))
                    dbg("cqT%d" % l, cqT[:, :, :], [('cqT', t) for t in range(NT)], [128, 2, S])
                    scale_b = 1.0 / float(np.sqrt(96.0))
                    for b in range(3):
                        if stop_after == ('B1', l):
                            done = True
                            break
                        h0 = 2 * b
                        QB, KB, VB = QBs[b % 2], KBs[b % 2], VBs[b % 2]
                        bb = b % 2
                        for t in range(NT):
                            bi = nextbank((0, 1))
                            for c in range(2):
                                mm(PF[bi][:, 0:192], cqT[:, c, t * 128:(t + 1) * 128], Wuq[:, c, h0 * 96:h0 * 96 + 192],
                                   c == 0, c == 1, ['Wuq', ('cqT', t)], [PK[bi]])
                            qv = PF[bi][:, 0:192].rearrange("p (h e) -> p h e", h=2)
                            qs = qst[t % 2]
                            qk = ('qst', t % 2)
                            rt1, rt2 = rt1s[t % 2], rt2s[t % 2]
                            r1k, r2k = ('rt1', t % 2), ('rt2', t % 2)
                            V('dve', 'tensor_copy', [PK[bi]], [qk], out=qs[:, :, 0:64], in_=qv[:, :, 0:64])
                            V('dve', 'tensor_tensor', [PK[bi], 'tab'], [r1k], out=rt1[:], in0=qv[:, :, 64:96],
                              in1=cc_t[:, t, :].unsqueeze(1).to_broadcast([128, 2, 32]), op=ALU.mult)
                            V('dve', 'tensor_tensor', [PK[bi], 'tab'], [r2k], out=rt2[:, :, 0:16], in0=qv[:, :, 80:96],
                              in1=ss_t[:, t, 0:16].unsqueeze(1).to_broadcast([128, 2, 16]), op=ALU.mult)
                            V('dve', 'tensor_tensor', [PK[bi], 'tab', r2k], [r2k], out=rt2[:, :, 16:32], in0=qv[:, :, 64:80],
                              in1=ss_t[:, t, 16:32].unsqueeze(1).to_broadcast([128, 2, 16]), op=ALU.mult)
                            V('pool', 'tensor_tensor', [r1k, r2k, qk], [qk], out=qs[:, :, 64:96], in0=rt1[:], in1=rt2[:],
                              op=ALU.add)
                            pi = 6 + (t % 2)
                            for hl in range(2):
                                tr(pbv(pi)[0:96, hl * 128:(hl + 1) * 128], qs[:, hl, :], [qk], [PK[pi]])
                            V('dve', 'tensor_copy', [PK[pi]], [('QB', bb, t // 4)], out=QB[0:96, :, t * 128:(t + 1) * 128],
                              in_=pbv(pi)[0:96, 0:256].rearrange("p (h k) -> p h k", h=2))
                        for hl in range(2):
                            h = h0 + hl
                            for tg in range(4):
                                bi = nextbank((0, 1))
                                mm(PF[bi][0:64, :], Wukv[:, h * 128:h * 128 + 64], ckvT[:, tg * 512:(tg + 1) * 512], True, True,
                                   ['Wukv'] + [('ckvT', 4 * tg + i) for i in range(4)], [PK[bi]])
                                V('dve', 'tensor_copy', [PK[bi]], [('KB', bb, tg)], out=KB[0:64, hl, tg * 512:(tg + 1) * 512],
                                  in_=PF[bi][0:64, :])
                        for t in range(NT):
                            bi = nextbank((0, 1))
                            for hl in range(2):
                                h = h0 + hl
                                mm(PF[bi][:, hl * 64:(hl + 1) * 64], ckvT[:, t * 128:(t + 1) * 128],
                                   Wukv[:, h * 128 + 64:h * 128 + 128], True, True, ['Wukv', ('ckvT', t)], [PK[bi]])
                            V('dve', 'tensor_copy', [PK[bi]], [('VB', 0, t)], out=VB[:, t, :, 0:64],
                              in_=PF[bi][:, 0:128].rearrange("p (h e) -> p h e", h=2))
                        if stop_after == ('B2', l):
                            done = True
                            break
                        heads = []
                        for hl in range(2):
                            heads.append(dict(
                                KT=lambda kt, hl=hl, KB=KB: KB[:, hl, kt * 128:(kt + 1) * 128],
                                QT=lambda q0, n, hl=hl, QB=QB: QB[:, hl, q0:q0 + n],
                                V=lambda kt, hl=hl, VB=VB: VB[:, kt, hl, :],
                                rk=lambda kt, qg, bb=bb: [('KB', bb, kt // 4), ('QB', bb, qg)] + [('KBr', bb, 4 * (kt // 4) + i) for i in range(4)],
                                vk=lambda kt: [('VB', 0, kt)]))
                        mx = mixed[b % 2]
                        mkey = ('mixed', b % 2)

                        def fin_b(qg, b=b, mx=mx, mkey=mkey):
                            for hl in range(2):
                                norm_out(2 + hl, mx[:, :, hl * 64:(hl + 1) * 64], [mkey], rden)
                            flush_chunk(mx, mkey, 3 + b, qg)
                        attend(sc, heads, 'B', scale_b, [2, 3], fin_b, stbanks=(0, 1, 4, 5, 6, 7), look=5)
                    T.barrier()
                  dbg("mtB%d" % l, MT[:, 3:6, :], [MK(c, t) for c in range(3, 6) for t in range(NT)], [128, 3, S])
                if stop_after == ('B', l):
                    done = True

                if not done:
                  with ExitStack() as es:
                    sc['TB'] = [sbt(es, "TBc%d" % i, [128, 512], F32) for i in range(4)]
                    WC = sbt(es, "WC", [128, 8, 388], BF16)
                    QC2 = [sbt(es, "QC%d" % i, [128, S], BF16) for i in range(2)]
                    V('act', 'memzero', [], [('QC', 0, tg) for tg in range(4)], QC2[0][:])
                    V('dve', 'memset', [], [('QC', 1, tg) for tg in range(4)], QC2[1][:], 0.0)
                    KC = sbt(es, "KC", [128, S], BF16)
                    VC = sbt(es, "VC", [128, NT, 2, 65], BF16)
                    cumneg = sbt(es, "cumneg", [4, S], F32)
                    negb = sbt(es, "negb", [4, 1], F32)
                    sel4 = sbt(es, "sel4", [4, 4, 128], F32)
                    cumk = sbt(es, "cumk", [128, NT, 4], F32)
                    cumq = None
                    V('pool', 'memset', [], [('VC', t) for t in range(NT)], VC[:, :, :, 64:65], 1.0)
                    V('dve', 'tensor_copy', ['ident'], ['sel4'], out=sel4[:],
                      in_=ident_f[0:4, 0:4].unsqueeze(2).to_broadcast([4, 4, 128]))
                    T.dma('sp', negb[:], W["b_forget"][l].rearrange("(h o) -> h o", o=1), writes=['negb'])
                    V('dve', 'tensor_scalar', ['negb'], ['negb'], out=negb[:], in0=negb[:], scalar1=-1.0, scalar2=None,
                      op0=ALU.mult)
                    for b in range(2):
                        for i, c0 in enumerate([C_QC, C_KC, C_VC]):
                            wload(WC[:, :, i * 128:(i + 1) * 128], win_cols(l, c0 + b * 128, 128), 'WC%d' % i)
                        if b == 0:
                            wload(WC[:, :, 384:388], win_cols(l, C_FC, 4), 'WC3')
                            esf = ExitStack()
                            lf = sbt(esf, "lf", [4, S], F32)
                            def evac_f(tg, bi):
                                V('act', 'activation', [PK[bi], 'negb'], [('lf', tg)], out=lf[:, tg * 512:(tg + 1) * 512],
                                  in_=PF[bi][0:4, :], func=AF.Exp, scale=-1.0, bias=negb[:, 0:1])
                                V('act', 'activation', [('lf', tg)], [('lf', tg)], out=lf[:, tg * 512:(tg + 1) * 512],
                                  in_=lf[:, tg * 512:(tg + 1) * 512], func=AF.Ln, bias=1.0)
                            proj_fm(WC, 'WC3', 384, 4, evac_f)
                            V('dve', 'tensor_tensor_scan', [('lf', i) for i in range(4)] + ['one1'], ['cumneg'],
                              out=cumneg[:], data0=one1[0:4, 0:1].to_broadcast([4, S]), data1=lf[:], initial=0.0, op0=ALU.mult, op1=ALU.add)
                            dbg("cumneg%d" % l, cumneg[:, :], ['cumneg'], [4, S])
                            bi = nextbank((0, 1))
                            for t in range(NT):
                                mm(PF[bi][:, t * 4:(t + 1) * 4], cumneg[0:4, t * 128:(t + 1) * 128], ident_f[0:4, 0:4],
                                   True, True, ['cumneg', 'ident'], [PK[bi]])
                            V('dve', 'tensor_copy', [PK[bi]], ['cumk'], out=cumk[:],
                              in_=PF[bi][:, 0:64].rearrange("p (t h) -> p t h", h=4))
                            esf.close()
                            cumq = [sbt(es, "cumq%d" % i, [128, S], F32) for i in range(2)]
                        for hl in range(2):
                            for tg in range(4):
                                bi = nextbank((0, 1))
                                mm(PF[bi][:, :], sel4[0:4, 2 * b + hl, :], cumneg[0:4, tg * 512:(tg + 1) * 512], True, True,
                                   ['sel4', 'cumneg'], [PK[bi]])
                                V('dve', 'tensor_copy', [PK[bi]], [('cumq', hl)], out=cumq[hl][:, tg * 512:(tg + 1) * 512],
                                  in_=PF[bi][:, :])
                        def evac_qc(tg, bi):
                            for i in range(2):
                                V('act', 'mul', [PK[bi]], [('QC', i, tg)], out=QC2[i][64 * i:64 * i + 64, tg * 512:(tg + 1) * 512],
                                  in_=PF[bi][64 * i:64 * i + 64, :], mul=0.125)
                        proj_fm(WC, 'WC0', 0, 128, evac_qc)
                        proj_fm(WC, 'WC1', 128, 128, lambda tg, bi: V('act', 'copy', [PK[bi]], [('KC', tg)],
                                out=KC[:, tg * 512:(tg + 1) * 512], in_=PF[bi][:, :]))
                        proj_tm(WC, 'WC2', 256, 128, lambda t, bi: V('dve', 'tensor_copy', [PK[bi]], [('VC', t)],
                                out=VC[:, t, :, 0:64], in_=PF[bi][:, 0:128].rearrange("p (h e) -> p h e", h=2)))
                        heads = []
                        for hl in range(2):
                            heads.append(dict(
                                KT=lambda kt: KC[:, kt * 128:(kt + 1) * 128],
                                QT=lambda q0, n, hl=hl: QC2[hl][:, q0:q0 + n],
                                V=lambda kt, hl=hl: VC[:, kt, hl, :],
                                rk=lambda kt, qg, hl=hl: [('KC', kt // 4), ('QC', hl, qg)],
                                vk=lambda kt: [('VC', kt)],
                                cumq=cumq[hl], cqk=('cumq', hl),
                                cumk=lambda kt, hh=2 * b + hl: cumk[:, kt, hh:hh + 1]))
                        mx = mixed[b % 2]
                        mkey = ('mixed', b % 2)

                        def fin_c(qg, b=b, mx=mx, mkey=mkey):
                            for hl in range(2):
                                norm_out(2 + hl, mx[:, :, hl * 64:(hl + 1) * 64], [mkey], rden)
                            flush_chunk(mx, mkey, 6 + b, qg)
                        attend(sc, heads, 'C', 1.0, [2, 3], fin_c, stbanks=(0, 1, 4, 5, 6, 7), look=5)
                    T.barrier()
                dbg("mt%d" % l, MT[:, :, :], [MK(c, t) for c in range(8) for t in range(NT)], [128, 8, S])
                if stop_after == ('C', l):
                    done = True
            if done:
                break
            T.barrier()

            esL = ExitStack()
            combT = sbt(esL, "combT%d" % l, [64, S], BF16)
            with ExitStack() as es:
                Wo = sbt(es, "Wo", [128, 8, DM], BF16)
                for half in range(2):
                    wload(Wo[:, :, half * 512:(half + 1) * 512],
                          W["w_out"][l].rearrange("(c p) n -> p c n", p=128)[:, :, half * 512:(half + 1) * 512], ('Wo', half))
                Wpg = sbt(es, "Wpg", [128, 8, DM], BF16)
                Wpp = sbt(es, "Wpp", [128, 2, DM], BF16)
                wload(Wpp[:], W["w_ple_proj"][l].rearrange("(c p) n -> p c n", p=128), 'Wpp')
                for hf in range(2):
                    wload(Wpg[:, :, hf * 512:(hf + 1) * 512],
                          W["w_ple_gate"][l].rearrange("(c p) n -> p c n", p=128)[:, :, hf * 512:(hf + 1) * 512], ('Wpg', hf))
                for half in range(2):
                    for t in range(NT):
                        bi = nextbank((0, 1, 2, 3))
                        for c in range(8):
                            mm(PF[bi][:, :], MT[:, c, t * 128:(t + 1) * 128], Wo[:, c, half * 512:(half + 1) * 512], c == 0,
                               c == 7, [('Wo', half), MK(c, t)], [PK[bi]])
                        V('dve', 'scalar_tensor_tensor', [PK[bi], RK(t)], [RK(t)], out=R[:, t, half * 512:(half + 1) * 512],
                          in0=R[:, t, half * 512:(half + 1) * 512], scalar=ALPHA, in1=PF[bi][:, :], op0=ALU.mult, op1=ALU.add)
                layer_norm(es, "ln1_g", "ln1_b", l, "1", ntmp=1)
                dbg("h%d" % l, R[:, :, :], [RK(t) for t in range(NT)], [128, NT, DM])
                build_xt(es, "h%d" % l)
                Wr = sbt(es, "Wr", [128, 8, 36], BF16)
                rb = sbt(es, "rb", [128, 36], F32)
                lg = sbt(es, "lg", [128, 36], F32)
                em = sbt(es, "em", [128, 32], F32)
                ex = sbt(es, "ex", [128, 32], F32)
                selm = sbt(es, "selm", [128, 32], F32)
                m8 = sbt(es, "m8", [128, 8], F32)
                sm = sbt(es, "sm", [128, 8], F32)
                t4 = sbt(es, "t4", [128, 4], F32)
                oh = sbt(es, "oh", [128, 4], F32)
                chl = [sbt(es, "chl%d" % i, [128, 64], BF16) for i in range(2)]
                chf = sbt(es, "chf", [128, 32], F32)
                wload(Wr[:, :, 0:4], W["w_group"][l].rearrange("(c p) n -> p c n", p=128), 'Wr0')
                for g in range(4):
                    wload(Wr[:, :, 4 + 8 * g:12 + 8 * g], W["w_erouter"][l, g].rearrange("(c p) n -> p c n", p=128),
                          'Wr%d' % (g + 1))
                T.dma('sp', rb[:, 0:4], W["b_group"][l].partition_broadcast(128), writes=['rb0'])
                T.dma('sp', rb[:, 4:36], W["b_erouter"][l].partition_broadcast(128), writes=['rb1'])
                wk = ['Wr%d' % i for i in range(5)]
                for t in range(NT):
                    bi = nextbank((0, 1))
                    for c in range(8):
                        mm(PF[bi][:, 0:36], XT[:, c, t * 128:(t + 1) * 128], Wr[:, c, :], c == 0, c == 7, wk + [XK(t)],
                           [PK[bi]])
                    V('dve', 'tensor_tensor', [PK[bi], 'rb0', 'rb1'], ['lg'], out=lg[:], in0=PF[bi][:, 0:36], in1=rb[:],
                      op=ALU.add)
                    V('dve', 'reduce_max', ['lg'], ['sm0'], out=sm[:, 0:1], in_=lg[:, 0:4], axis=AX.X)
                    V('dve', 'tensor_scalar', ['lg', 'sm0'], ['oh'], out=oh[:], in0=lg[:, 0:4], scalar1=sm[:, 0:1], scalar2=None,
                      op0=ALU.is_equal)
                    V('dve', 'tensor_scalar', ['lg', 'sm0'], ['t4'], out=t4[:], in0=lg[:, 0:4], scalar1=sm[:, 0:1], scalar2=None,
                      op0=ALU.subtract)
                    V('pool', 'memset', [], ['sm1'], sm[:, 1:2], 0.0)
                    V('act', 'activation', ['t4'], ['t4', 'sm1'], out=t4[:], in_=t4[:], func=AF.Exp, accum_out=sm[:, 1:2])
                    V('dve', 'tensor_scalar', ['oh'], ['oh'], out=oh[:], in0=oh[:], scalar1=1e9, scalar2=-1e9, op0=ALU.mult,
                      op1=ALU.add)
                    V('dve', 'tensor_tensor', ['lg', 'oh'], ['em'], out=em[:].rearrange("p (g e) -> p g e", g=4),
                      in0=lg[:, 4:36].rearrange("p (g e) -> p g e", g=4), in1=oh[:].unsqueeze(2).to_broadcast([128, 4, 8]),
                      op=ALU.add)
                    V('dve', 'max', ['em'], ['m8'], out=m8[:], in_=em[:])
                    V('dve', 'tensor_scalar', ['em', 'm8'], ['selm'], out=selm[:], in0=em[:], scalar1=m8[:, 1:2], scalar2=None,
                      op0=ALU.is_ge)
                    V('dve', 'tensor_scalar', ['em', 'm8'], ['ex'], out=ex[:], in0=em[:], scalar1=m8[:, 0:1], scalar2=None,
                      op0=ALU.subtract)
                    V('act', 'activation', ['ex'], ['ex'], out=ex[:], in_=ex[:], func=AF.Exp)
                    V('dve', 'tensor_tensor', ['ex', 'selm'], ['ex'], out=ex[:], in0=ex[:], in1=selm[:], op=ALU.mult)
                    V('dve', 'reduce_sum', ['ex'], ['sm2'], out=sm[:, 2:3], in_=ex[:], axis=AX.X)
                    V('dve', 'tensor_tensor', ['sm1', 'sm2'], ['sm3'], out=sm[:, 3:4], in0=sm[:, 1:2], in1=sm[:, 2:3],
                      op=ALU.mult)
                    V('dve', 'reciprocal', ['sm3'], ['sm3'], out=sm[:, 3:4], in_=sm[:, 3:4])
                    V('dve', 'tensor_scalar', ['ex', 'sm3'], ['ex'], out=ex[:], in0=ex[:], scalar1=sm[:, 3:4], scalar2=None,
                      op0=ALU.mult)
                    ch = chl[t % 2]
                    ck = ('chl', t % 2)
                    V('dve', 'tensor_copy', ['ex'], [ck], out=ch[:, 0:32], in_=ex[:])
                    V('dve', 'tensor_copy', [ck], ['chf'], out=chf[:], in_=ch[:, 0:32])
                    V('dve', 'tensor_tensor', ['ex', 'chf', ck], [ck], out=ch[:, 32:64], in0=ex[:], in1=chf[:], op=ALU.subtract)
                    pi = 6 + (t % 2)
                    tr(pbv(pi)[0:64, 0:128], ch[:, :], [ck], [PK[pi]])
                    V('act', 'copy', [PK[pi]], [('combT', t)], out=combT[:, t * 128:(t + 1) * 128], in_=pbv(pi)[0:64, 0:128])
                dbg("combT%d" % l, combT[:, :], [('combT', t) for t in range(NT)], [64, S])
                ptm = [sbt(es, "ptm%d" % i, [128, 256], BF16) for i in range(2)]
                pTt = [sbt(es, "pTt%d" % i, [128, 2, 128], BF16) for i in range(2)]
                sg = [sbt(es, "sg%d" % i, [128, 512], F32) for i in range(2)]
                cnt = 0
                for t in range(NT):
                    pm = ptm[t % 2]
                    pk = ('ptm', t % 2)
                    wload(pm[:], p_d[l, t * 128:(t + 1) * 128, :], pk)
                    pi = 6 + (t % 2)
                    for c in range(2):
                        tr(pbv(pi)[:, c * 128:(c + 1) * 128], pm[:, c * 128:(c + 1) * 128], [pk], [PK[pi]])
                    pt = pTt[t % 2]
                    ptk = ('pTt', t % 2)
                    V('act', 'copy', [PK[pi]], [ptk], out=pt[:], in_=pbv(pi)[:, 0:256].rearrange("p (c k) -> p c k", c=2))
                    for half in range(2):
                        bg = nextbank((0, 1, 4))
                        bp = nextbank((2, 3, 5))
                        for c in range(8):
                            mm(PF[bg][:, :], XT[:, c, t * 128:(t + 1) * 128], Wpg[:, c, half * 512:(half + 1) * 512], c == 0,
                               c == 7, [('Wpg', half), XK(t)], [PK[bg]])
                        for c in range(2):
                            mm(PF[bp][:, :], pt[:, c, :], Wpp[:, c, half * 512:(half + 1) * 512], c == 0, c == 1,
                               ['Wpp', ptk], [PK[bp]])
                        s_ = sg[cnt % 2]
                        sk = ('sg', cnt % 2)
                        cnt += 1
                        V('act', 'activation', [PK[bg]], [sk], out=s_[:], in_=PF[bg][:, :], func=AF.Sigmoid)
                        V('dve', 'tensor_tensor', [sk, PK[bp]], [sk], out=s_[:], in0=s_[:], in1=PF[bp][:, :], op=ALU.mult)
                        V('dve', 'scalar_tensor_tensor', [sk, RK(t)], [RK(t)], out=R[:, t, half * 512:(half + 1) * 512],
                          in0=R[:, t, half * 512:(half + 1) * 512], scalar=ALPHA, in1=s_[:], op0=ALU.mult, op1=ALU.add)
                dbg("rple%d" % l, R[:, :, :], [RK(t) for t in range(NT)], [128, NT, DM])
            T.barrier()
            if stop_after == ('PLE', l):
                esL.close()
                break

            with ExitStack() as es:
                WG = [sbt(es, "WG%d" % i, [128, 8, 256], BF16) for i in range(2)]
                WU = [sbt(es, "WU%d" % i, [128, 8, 256], BF16) for i in range(2)]
                WD = [sbt(es, "WD%d" % i, [128, 2, DM], BF16) for i in range(4)]
                sel2 = sbt(es, "sel2", [64, 32, 128], BF16)
                cbs = [sbt(es, "cbs%d" % i, [128, 512], BF16) for i in range(2)]
                sl = [sbt(es, "sl%d" % i, [128, 512], BF16) for i in range(2)]
                tu = [sbt(es, "tu%d" % i, [128, 512], BF16) for i in range(2)]
                V('dve', 'tensor_tensor', ['ident'], ['sel2'], out=sel2[:],
                  in0=ident_b[0:64, 0:32].unsqueeze(2).to_broadcast([64, 32, 128]),
                  in1=ident_b[0:64, 32:64].unsqueeze(2).to_broadcast([64, 32, 128]), op=ALU.add)

                def load_expert(e):
                    s_ = e % 2
                    wload(WG[s_][:], W["w_gate_e"][l, e].rearrange("(c p) n -> p c n", p=128), ('WG', s_))
                    wload(WU[s_][:], W["w_up_e"][l, e].rearrange("(c p) n -> p c n", p=128), ('WU', s_))
                load_expert(0)
                cn2 = 0
                for eb in range(8):
                    for el in range(4):
                        e = eb * 4 + el
                        if e + 1 < 32:
                            load_expert(e + 1)
                        wload(WD[el][:], W["w_down_e"][l, e].rearrange("(c p) n -> p c n", p=128), ('WD', el))
                        s_ = e % 2
                        for tg in range(4):
                            cb = cbs[tg % 2]
                            cbk = ('cbs', tg % 2)
                            mm(PF[4][:, :], sel2[:, e, :], combT[:, tg * 512:(tg + 1) * 512], True, True,
                               ['sel2'] + [('combT', 4 * tg + i) for i in range(4)], [PK[4]])
                            V('act', 'copy', [PK[4]], [cbk], out=cb[:], in_=PF[4][:, :])
                            for fc in range(2):
                                bg, bu = (0, 1) if cn2 % 2 == 0 else (2, 3)
                                k2 = cn2 % 2
                                cn2 += 1
                                for c in range(8):
                                    mm(PF[bg][:, :], WG[s_][:, c, fc * 128:(fc + 1) * 128], XT[:, c, tg * 512:(tg + 1) * 512],
                                       c == 0, c == 7, [('WG', s_)] + [XK(4 * tg + i) for i in range(4)], [PK[bg]])
                                for c in range(8):
                                    mm(PF[bu][:, :], WU[s_][:, c, fc * 128:(fc + 1) * 128], XT[:, c, tg * 512:(tg + 1) * 512],
                                       c == 0, c == 7, [('WU', s_)] + [XK(4 * tg + i) for i in range(4)], [PK[bu]])
                                V('act', 'activation', [PK[bg]], [('sl', k2)], out=sl[k2][:], in_=PF[bg][:, :], func=AF.Silu)
                                V('dve', 'tensor_tensor', [PK[bu], ('sl', k2)], [('tu', k2)], out=tu[k2][:], in0=PF[bu][:, :],
                                  in1=sl[k2][:], op=ALU.mult)
                                V('pool', 'tensor_tensor', [('tu', k2), cbk], [MK(el * 2 + fc, 4 * tg + i) for i in range(4)],
                                  out=MT[:, el * 2 + fc, tg * 512:(tg + 1) * 512], in0=tu[k2][:], in1=cb[:], op=ALU.mult)
                    for t in range(NT):
                        for half in range(2):
                            bi = nextbank((5, 6, 7))
                            for sl_ in range(8):
                                mm(PF[bi][:, :], MT[:, sl_, t * 128:(t + 1) * 128],
                                   WD[sl_ // 2][:, sl_ % 2, half * 512:(half + 1) * 512], sl_ == 0, sl_ == 7,
                                   [('WD', sl_ // 2), MK(sl_, t)], [PK[bi]])
                            V('dve', 'tensor_tensor', [PK[bi], RK(t)], [RK(t)], out=R[:, t, half * 512:(half + 1) * 512],
                              in0=R[:, t, half * 512:(half + 1) * 512], in1=PF[bi][:, :], op=ALU.add)
                dbg("rmoe%d" % l, R[:, :, :], [RK(t) for t in range(NT)], [128, NT, DM])
            T.barrier()
            with ExitStack() as es:
                last = (l == nlayers - 1)

                def store(t):
                    if last:
                        T.dma('sp', out_d[t * 128:(t + 1) * 128, :], R[:, t, :], reads=[RK(t)], is_out=True)
                layer_norm(es, "ln2_g", "ln2_b", l, "2", after=store)
                if not last:
                    build_xt(es, "n%d" % l)
            T.barrier()
            esL.close()
        T.emit()
    return nc, dbg_d, T.stats


def host_consts():
    inv = (10000.0 ** (-np.arange(16, dtype=np.float32) / np.float32(16))).astype(np.float32)
    return (inv / np.float32(2 * np.pi)).astype(np.float32)


_CACHE = {}


def make_in_maps(inputs, ncores=8):
    cst = host_consts()
    maps = []
    for c in range(ncores):
        m = {"x": np.ascontiguousarray(inputs["x"][c]), "p": np.ascontiguousarray(inputs["p"][:, c]),
             "positions": np.ascontiguousarray(inputs["positions"][c]).astype(np.int32), "cst": cst}
        for n, sh in WNAMES:
            m[n] = np.ascontiguousarray(np.asarray(inputs[n], dtype=np.float32).reshape(sh))
        maps.append(m)
    return maps


def kernel(**inputs):
    inputs = {k: np.asarray(v) for k, v in inputs.items()}
    if 'nc' not in _CACHE:
        _CACHE['nc'] = build()[0]
    nc = _CACHE['nc']
    maps = make_in_maps(inputs)
    res = run_bass_kernel_spmd(nc, maps, core_ids=list(range(8)))
    return np.stack([np.asarray(r["out"], dtype=np.float32) for r in res.results], axis=0)
```
